# Optimizing a Trainium2 kernel written in Bass

```python
import jax, jax.numpy as jnp
from jax import lax
import numpy as np

D_MODEL = 2048
BATCH = 4
SEQ = 2048
DEPTH = 2
DEC_BATCH = 128
DEC_SEQ = 4
PAST_LEN = 16384
PAGE_SIZE = 128

N_EVEN = (DEPTH + 1) // 2
N_ODD = DEPTH // 2
MIX_WIDTH = 2 * D_MODEL
A_INNER = MIX_WIDTH // 2
A_HEAD_DIM = 64
A_HEADS = A_INNER // A_HEAD_DIM
A_GROUPS = 8
A_STATE = 128
A_CONV_W = 4
A_CONV_DIM = A_INNER + 2 * A_GROUPS * A_STATE
A_CHUNK = 128
B_HEADS = 4
B_VALUE = MIX_WIDTH // 2
B_KEY = B_VALUE // 2
B_DK = B_KEY // B_HEADS
B_DV = B_VALUE // B_HEADS
B_GATE_RANK = 16
B_GATE_TAU = 16.0
B_CHUNK = 64
IN0_DIM = A_INNER + A_CONV_DIM + A_HEADS + 2 * B_KEY + 2 * B_VALUE + B_GATE_RANK
C_WIDTH = MIX_WIDTH
C_GROUPS = 8
C_GROUP_DIM = C_WIDTH // C_GROUPS
C_CHUNK = 128
D_FF = 64 * ((-(-8 * D_MODEL // 3)) // 64 + (1 if (-(-8 * D_MODEL // 3)) % 64 else 0))
N_EXPERTS = 8
TOP_K = 2
EPS = 1e-6

kernel_name = 'hybrid_ssd_gla_chunkmlp_moe_step'


def rmsnorm(x, g):
    xf = x.astype(jnp.float32)
    y = xf * lax.rsqrt(jnp.mean(xf * xf, axis=-1, keepdims=True) + EPS)
    return (y * g.astype(jnp.float32)).astype(x.dtype)


def adaln(c, w, b):
    mod = jax.nn.silu(c) @ w + b
    return jnp.split(mod, 6, axis=-1)


def modulate(h, shift, scale):
    return h * (1.0 + scale[:, None]) + shift[:, None]


def split_sizes(a, sizes):
    idx = [int(i) for i in np.cumsum(sizes)[:-1]]
    return jnp.split(a, idx, axis=-1)


def to_chunks(t, cl, nc):
    pad = nc * cl - t.shape[1]
    t = jnp.pad(t, [(0, 0), (0, pad)] + [(0, 0)] * (t.ndim - 2))
    return t.reshape(t.shape[0], nc, cl, *t.shape[2:])


def swiglu(h, w1, w3, w2):
    return (jax.nn.silu(h @ w1) * (h @ w3)) @ w2


def causal_conv(xbc, buf, w, b):
    full = jnp.concatenate([buf.astype(xbc.dtype), xbc], axis=1)
    y = lax.conv_general_dilated(full, w[:, None, :].astype(full.dtype), (1,), 'VALID',
                                 dimension_numbers=('NWC', 'WIO', 'NWC'),
                                 feature_group_count=A_CONV_DIM) + b
    new_buf = full[:, full.shape[1] - (A_CONV_W - 1):]
    return jax.nn.silu(y), new_buf


def ssd_scan(x, dt, a, bm, cm, h0):
    bsz, L = x.shape[:2]
    g, r = A_GROUPS, A_HEADS // A_GROUPS
    cl = min(A_CHUNK, L)
    nc = -(-L // cl)
    xdt = to_chunks((x * dt[..., None]).reshape(bsz, L, g, r, A_HEAD_DIM), cl, nc)
    da = to_chunks((dt * a).reshape(bsz, L, g, r), cl, nc)
    bc = to_chunks(bm, cl, nc)
    cc = to_chunks(cm, cl, nc)
    acum = jnp.cumsum(da, axis=2)
    causal = jnp.tril(jnp.ones((cl, cl), bool))
    seg = acum[:, :, :, None] - acum[:, :, None, :]
    decay = jnp.exp(jnp.where(causal[:, :, None, None], seg, -jnp.inf))
    cb = jnp.einsum('bctgn,bcsgn->bctsg', cc, bc)
    y_diag = jnp.einsum('bctsgr,bcsgrp->bctgrp', cb[..., None] * decay, xdt)
    to_end = jnp.exp(acum[:, :, -1:] - acum)
    states = jnp.einsum('bcsgn,bcsgrp->bcgrpn', bc, xdt * to_end[..., None])
    chunk_decay = jnp.exp(acum[:, :, -1])

    def step(hs, inp):
        st, dec = inp
        return hs * dec[..., None, None] + st, hs

    h_last, h_prev = lax.scan(step, h0.reshape(bsz, g, r, A_HEAD_DIM, A_STATE),
                              (jnp.moveaxis(states, 1, 0), jnp.moveaxis(chunk_decay, 1, 0)))
    y_off = jnp.einsum('bctgn,cbgrpn->bctgrp', cc, h_prev) * jnp.exp(acum)[..., None]
    y = (y_diag + y_off).reshape(bsz, nc * cl, A_HEADS, A_HEAD_DIM)[:, :L]
    return y, h_last.reshape(bsz, A_HEADS, A_HEAD_DIM, A_STATE)


def gla_chunked(q, k, v, log_a, s0):
    bsz, L = q.shape[:2]
    cl = min(B_CHUNK, L)
    nc = -(-L // cl)
    qc, kc, vc, gc = (to_chunks(t, cl, nc) for t in (q, k, v, log_a))
    bcum = jnp.cumsum(gc, axis=2)
    q_t = qc * jnp.exp(bcum)
    k_t = kc * jnp.exp(-bcum)
    causal = jnp.tril(jnp.ones((cl, cl), bool))
    att = jnp.where(causal, jnp.einsum('bcthk,bcshk->bchts', q_t, k_t), 0.0)
    o_intra = jnp.einsum('bchts,bcshv->bcthv', att, vc)
    b_last = bcum[:, :, -1]
    ds = jnp.einsum('bcshk,bcshv->bchkv', kc * jnp.exp(b_last[:, :, None] - bcum), vc)

    def step(s, inp):
        d_s, bl = inp
        return s * jnp.exp(bl)[..., None] + d_s, s

    s_last, s_prev = lax.scan(step, s0, (jnp.moveaxis(ds, 1, 0), jnp.moveaxis(b_last, 1, 0)))
    o_inter = jnp.einsum('bcthk,cbhkv->bcthv', q_t, s_prev)
    o = (o_intra + o_inter).reshape(bsz, nc * cl, B_HEADS, B_DV)[:, :L]
    return o, s_last


def mixer_ab(h, ssm0, conv0, gla0, w_in, conv_w, conv_b, dt_bias, a_log, d_skip, a_norm,
             gla_wa2, gla_ba, gla_norm, w_out):
    f32 = jnp.float32
    bsz, L, _ = h.shape
    z, xbc, dt_raw, q, k, v, r, g_lr = split_sizes(
        h @ w_in, [A_INNER, A_CONV_DIM, A_HEADS, B_KEY, B_KEY, B_VALUE, B_VALUE, B_GATE_RANK])
    xbc_c, conv_new = causal_conv(xbc, conv0, conv_w, conv_b)
    xs, bm, cm = split_sizes(xbc_c, [A_INNER, A_GROUPS * A_STATE, A_GROUPS * A_STATE])
    xs = xs.reshape(bsz, L, A_HEADS, A_HEAD_DIM).astype(f32)
    dt = jax.nn.softplus(dt_raw.astype(f32) + dt_bias.astype(f32))
    a = -jnp.exp(a_log.astype(f32))
    y_a, ssm_new = ssd_scan(xs, dt, a,
                            bm.reshape(bsz, L, A_GROUPS, A_STATE).astype(f32),
                            cm.reshape(bsz, L, A_GROUPS, A_STATE).astype(f32),
                            ssm0.astype(f32))
    y_a = (y_a + d_skip.astype(f32)[:, None] * xs).reshape(bsz, L, A_INNER)
    y_a = rmsnorm(y_a * jax.nn.silu(z.astype(f32)), a_norm)
    log_a = jax.nn.log_sigmoid((g_lr @ gla_wa2 + gla_ba).astype(f32)) / B_GATE_TAU
    qh = q.reshape(bsz, L, B_HEADS, B_DK).astype(f32) * (B_DK ** -0.5)
    kh = k.reshape(bsz, L, B_HEADS, B_DK).astype(f32)
    vh = v.reshape(bsz, L, B_HEADS, B_DV).astype(f32)
    o, gla_new = gla_chunked(qh, kh, vh, log_a.reshape(bsz, L, B_HEADS, B_DK), gla0.astype(f32))
    o = rmsnorm(o, gla_norm.reshape(B_HEADS, B_DV))
    y_b = o.reshape(bsz, L, B_VALUE) * jax.nn.silu(r.astype(f32))
    y = jnp.concatenate([y_a, y_b], axis=-1).astype(h.dtype) @ w_out
    return y, ssm_new.astype(ssm0.dtype), conv_new.astype(conv0.dtype), gla_new.astype(gla0.dtype)


def chunk_mix(v, ws, bs):
    bsz, L = v.shape[:2]
    cl = min(C_CHUNK, L)
    nc = -(-L // cl)
    vp = to_chunks(v, cl, nc)
    w = jnp.tril(ws[:, :cl, :cl]).astype(v.dtype)
    y = jnp.einsum('gts,bnsge->bntge', w, vp) + bs[:, :cl].T.astype(v.dtype)[None, None, :, :, None]
    return y.reshape(bsz, nc * cl, C_GROUPS, C_GROUP_DIM)[:, :L]


def mixer_c(h, w_in, ln_g, ln_b, ws, bs, w_out):
    f32 = jnp.float32
    bsz, L, _ = h.shape
    u, v = jnp.split(jax.nn.gelu(h @ w_in), 2, axis=-1)
    vf = v.reshape(bsz, L, C_GROUPS, C_GROUP_DIM).astype(f32)
    mu = jnp.mean(vf, axis=-1, keepdims=True)
    var = jnp.mean(jnp.square(vf - mu), axis=-1, keepdims=True)
    vn = (vf - mu) * lax.rsqrt(var + EPS) * ln_g.reshape(C_GROUPS, C_GROUP_DIM).astype(f32) \
        + ln_b.reshape(C_GROUPS, C_GROUP_DIM).astype(f32)
    mixed = chunk_mix(vn, ws.astype(f32), bs.astype(f32))
    y = (u.reshape(bsz, L, C_GROUPS, C_GROUP_DIM).astype(f32) * mixed).reshape(bsz, L, C_WIDTH)
    return y.astype(h.dtype) @ w_out, vn.astype(h.dtype)


def moe(h, router_w, w1, w3, w2):
    bsz, L, d = h.shape
    t = h.reshape(-1, d)
    logits = (t @ router_w).astype(jnp.float32)
    top_v, top_i = lax.top_k(logits, TOP_K)
    gates = jax.nn.softmax(top_v, axis=-1)
    dense_gate = jnp.sum(jax.nn.one_hot(top_i, N_EXPERTS, dtype=jnp.float32) * gates[..., None], axis=1)
    y = jnp.zeros(t.shape, jnp.float32)
    for e in range(N_EXPERTS):
        y = y + dense_gate[:, e:e + 1] * swiglu(t, w1[e], w3[e], w2[e]).astype(jnp.float32)
    return y.astype(h.dtype).reshape(bsz, L, d)


def even_layer(x, c, ssm0, conv0, gla0, P, i):
    sm, scm, gm, sf, scf, gf = adaln(c, P['ada_w0'][i], P['ada_b0'][i])
    h = modulate(rmsnorm(x, P['norm_mix0'][i]), sm, scm)
    y, ssm_new, conv_new, gla_new = mixer_ab(
        h, ssm0, conv0, gla0, P['w_in0'][i], P['conv_w'][i], P['conv_b'][i], P['dt_bias'][i],
        P['a_log'][i], P['d_skip'][i], P['a_norm'][i], P['gla_wa2'][i], P['gla_ba'][i],
        P['gla_norm'][i], P['w_out0'][i])
    x = x + gm[:, None] * y
    h = modulate(rmsnorm(x, P['norm_ffn0'][i]), sf, scf)
    x = x + gf[:, None] * swiglu(h, P['ffn_w1'][i], P['ffn_w3'][i], P['ffn_w2'][i])
    return x, ssm_new, conv_new, gla_new


def odd_layer(x, c, P, i):
    sm, scm, gm, sf, scf, gf = adaln(c, P['ada_w1'][i], P['ada_b1'][i])
    h = modulate(rmsnorm(x, P['norm_mix1'][i]), sm, scm)
    y, v_rows = mixer_c(h, P['c_w_in'][i], P['c_ln_g'][i], P['c_ln_b'][i], P['c_ws'][i],
                        P['c_bs'][i], P['c_w_out'][i])
    x = x + gm[:, None] * y
    h = modulate(rmsnorm(x, P['norm_ffn1'][i]), sf, scf)
    x = x + gf[:, None] * moe(h, P['router_w'][i], P['moe_w1'][i], P['moe_w3'][i], P['moe_w2'][i])
    return x, v_rows


def run_trunk(x, c, ssm_in, conv_in, gla_in, P):
    ssm_out, conv_out, gla_out, v_out = [], [], [], []
    for layer in range(DEPTH):
        i = layer // 2
        if layer % 2 == 0:
            x, s, cv, g = even_layer(x, c, ssm_in[i], conv_in[i], gla_in[i], P, i)
            ssm_out.append(s)
            conv_out.append(cv)
            gla_out.append(g)
        else:
            x, vr = odd_layer(x, c, P, i)
            v_out.append(vr)
    return rmsnorm(x, P['norm_f']), ssm_out, conv_out, gla_out, v_out


def setup_inputs(seed: int = 0) -> dict:
    key = jax.random.key(seed)
    ks = iter(jax.random.split(key, 48))
    f32 = jnp.float32

    def nrm(shape, scale):
        return jax.random.normal(next(ks), shape, f32) * scale

    def gain(shape):
        return 1.0 + nrm(shape, 0.02)

    D = D_MODEL
    dt0 = jnp.exp(jax.random.uniform(next(ks), (N_EVEN, A_HEADS), f32, np.log(1e-3), np.log(1e-1)))
    inp = {
        'x_prompt': nrm((BATCH, SEQ, D), 1.0),
        'x_sample': nrm((DEC_BATCH, DEC_SEQ, D), 1.0),
        'state_ssm': nrm((N_EVEN, DEC_BATCH, A_HEADS, A_HEAD_DIM, A_STATE), 0.1),
        'state_conv': nrm((N_EVEN, DEC_BATCH, A_CONV_W - 1, A_CONV_DIM), 1.0),
        'state_gla': nrm((N_EVEN, DEC_BATCH, B_HEADS, B_DK, B_DV), 0.5),
        'c_prompt': nrm((BATCH, D), 1.0),
        'c_sample': nrm((DEC_BATCH, D), 1.0),
        'ada_w0': nrm((N_EVEN, D, 6 * D), 0.5 * D ** -0.5),
        'ada_b0': nrm((N_EVEN, 6 * D), 0.02),
        'norm_mix0': gain((N_EVEN, D)),
        'norm_ffn0': gain((N_EVEN, D)),
        'w_in0': nrm((N_EVEN, D, IN0_DIM), D ** -0.5),
        'conv_w': nrm((N_EVEN, A_CONV_W, A_CONV_DIM), 0.5),
        'conv_b': nrm((N_EVEN, A_CONV_DIM), 0.02),
        'dt_bias': dt0 + jnp.log(-jnp.expm1(-dt0)),
        'a_log': jnp.log(jax.random.uniform(next(ks), (N_EVEN, A_HEADS), f32, 1.0, 16.0)),
        'd_skip': gain((N_EVEN, A_HEADS)),
        'a_norm': gain((N_EVEN, A_INNER)),
        'gla_wa2': nrm((N_EVEN, B_GATE_RANK, B_KEY), B_GATE_RANK ** -0.5),
        'gla_ba': nrm((N_EVEN, B_KEY), 0.1),
        'gla_norm': gain((N_EVEN, B_VALUE)),
        'w_out0': nrm((N_EVEN, MIX_WIDTH, D), MIX_WIDTH ** -0.5),
        'ffn_w1': nrm((N_EVEN, D, D_FF), D ** -0.5),
        'ffn_w3': nrm((N_EVEN, D, D_FF), D ** -0.5),
        'ffn_w2': nrm((N_EVEN, D_FF, D), D_FF ** -0.5),
        'ada_w1': nrm((N_ODD, D, 6 * D), 0.5 * D ** -0.5),
        'ada_b1': nrm((N_ODD, 6 * D), 0.02),
        'norm_mix1': gain((N_ODD, D)),
        'norm_ffn1': gain((N_ODD, D)),
        'c_w_in': nrm((N_ODD, D, 2 * C_WIDTH), D ** -0.5),
        'c_ln_g': gain((N_ODD, C_WIDTH)),
        'c_ln_b': nrm((N_ODD, C_WIDTH), 0.02),
        'c_ws': nrm((N_ODD, C_GROUPS, C_CHUNK, C_CHUNK), 0.5 * C_CHUNK ** -0.5),
        'c_bs': 1.0 + nrm((N_ODD, C_GROUPS, C_CHUNK), 0.1),
        'c_w_out': nrm((N_ODD, C_WIDTH, D), C_WIDTH ** -0.5),
        'router_w': nrm((N_ODD, D, N_EXPERTS), D ** -0.5),
        'moe_w1': nrm((N_ODD, N_EXPERTS, D, D_FF), D ** -0.5),
        'moe_w3': nrm((N_ODD, N_EXPERTS, D, D_FF), D ** -0.5),
        'moe_w2': nrm((N_ODD, N_EXPERTS, D_FF, D), D_FF ** -0.5),
        'norm_f': gain((D,)),
    }
    return inp


def reference(x_prompt, x_sample, state_ssm, state_conv, state_gla, c_prompt, c_sample,
              ada_w0, ada_b0, norm_mix0, norm_ffn0, w_in0, conv_w, conv_b, dt_bias, a_log, d_skip,
              a_norm, gla_wa2, gla_ba, gla_norm, w_out0, ffn_w1, ffn_w3, ffn_w2,
              ada_w1, ada_b1, norm_mix1, norm_ffn1, c_w_in, c_ln_g, c_ln_b, c_ws, c_bs, c_w_out,
              router_w, moe_w1, moe_w3, moe_w2, norm_f):
    P = dict(ada_w0=ada_w0, ada_b0=ada_b0, norm_mix0=norm_mix0, norm_ffn0=norm_ffn0, w_in0=w_in0,
             conv_w=conv_w, conv_b=conv_b, dt_bias=dt_bias, a_log=a_log, d_skip=d_skip, a_norm=a_norm,
             gla_wa2=gla_wa2, gla_ba=gla_ba, gla_norm=gla_norm, w_out0=w_out0, ffn_w1=ffn_w1,
             ffn_w3=ffn_w3, ffn_w2=ffn_w2, ada_w1=ada_w1, ada_b1=ada_b1, norm_mix1=norm_mix1,
             norm_ffn1=norm_ffn1, c_w_in=c_w_in, c_ln_g=c_ln_g, c_ln_b=c_ln_b, c_ws=c_ws, c_bs=c_bs,
             c_w_out=c_w_out, router_w=router_w, moe_w1=moe_w1, moe_w3=moe_w3, moe_w2=moe_w2,
             norm_f=norm_f)
    bp = x_prompt.shape[0]
    ssm_zero = jnp.zeros((N_EVEN, bp, A_HEADS, A_HEAD_DIM, A_STATE), x_prompt.dtype)
    conv_zero = jnp.zeros((N_EVEN, bp, A_CONV_W - 1, A_CONV_DIM), x_prompt.dtype)
    gla_zero = jnp.zeros((N_EVEN, bp, B_HEADS, B_DK, B_DV), x_prompt.dtype)
    y_prompt, ssm_p, conv_p, gla_p, _ = run_trunk(x_prompt, c_prompt, ssm_zero, conv_zero, gla_zero, P)
    y_sample, ssm_s, conv_s, gla_s, v_s = run_trunk(x_sample, c_sample, state_ssm, state_conv, state_gla, P)
    ssm_prompt = jnp.stack(ssm_p)
    conv_prompt = jnp.stack(conv_p)
    gla_prompt = jnp.stack(gla_p)
    ssm_sample = jnp.stack(ssm_s)
    conv_sample = jnp.stack(conv_s)
    gla_sample = jnp.stack(gla_s)
    cmlp_v_sample = jnp.stack(v_s)
    return (y_prompt, y_sample, ssm_prompt, conv_prompt, gla_prompt, ssm_sample, conv_sample, gla_sample, cmlp_v_sample)
```

```python
import contextlib
import numpy as np
import concourse.bass as bass
import concourse.mybir as mybir
from concourse.bass_utils import run_bass_kernel_spmd

F32 = mybir.dt.float32
BF16 = mybir.dt.bfloat16
AF = mybir.ActivationFunctionType
ALU = mybir.AluOpType

D = 2048
TP = 1024
TS = 64
TA = 2112
TM = 1088
NSEQ = 17
DFF = 5504
NIN = 12336
NEXP = 8
EPS = 1e-6
NSLOT = 4
SLOT_ELEMS = 4096

C_Z, C_XBC, C_DT, C_Q, C_K, C_V, C_R, C_G = 0, 2048, 6144, 6176, 7200, 8224, 10272, 12320

K_ID, K_L, K_R, K_CM, K_BLK, K_LS, K_RS, K_CMS, K_BLKS, K_SEQM, K_PICK, K_ONE = (
    0, 128, 256, 384, 512, 640, 768, 896, 1024, 1152, 1168, 1184)
K_SELP, K_SELS, K_EPS = 1312, 1440, 1504
NCONST = 1512
R_NM0, R_NF0, R_AN, R_GN, R_NM1, R_NF1, R_NFIN, R_LG0, R_LG1, R_LB0, R_LB1, R_BA = range(12)
NROW = 12


class Prog:
    def __init__(self, nc):
        self.nc = nc
        self.ops = []
        self.out_chans = set()
        self.all_chans = set()

    def add(self, eng, fn, r=(), w=(), chan=None):
        self.ops.append(dict(eng=eng, fn=fn, r=tuple(r), w=tuple(w), chan=chan))
        return len(self.ops) - 1

    def pe(self, fn, r=(), w=()):
        return self.add('pe', fn, r, w)

    def dve(self, fn, r=(), w=()):
        return self.add('dve', fn, r, w)

    def act(self, fn, r=(), w=()):
        return self.add('act', fn, r, w)

    def pool(self, fn, r=(), w=()):
        return self.add('pool', fn, r, w)

    def barrier(self):
        self.ops.append(dict(eng='bar', fn=None, r=(), w=(), chan=None))

    def dma(self, eng, out, in_, r=(), w=(), chan=None, is_out=False, mode_all=False, accum=False):
        if is_out:
            self.out_chans.add(chan)
        if mode_all:
            self.all_chans.add(chan)
        if accum:
            fn = lambda e, o=out, i=in_: e.dma_start(out=o, in_=i, accum_op=ALU.add)
        else:
            fn = lambda e, o=out, i=in_: e.dma_start(out=o, in_=i)
        return self.add(eng, fn, r, w, chan=chan)

    def emit(self):
        nc = self.nc
        ops = self.ops
        last_w = {}
        readers = {}
        last_eng = {}
        last_chan = {}
        pend = {}
        for i, op in enumerate(ops):
            raw = set()
            deps = set()
            if op['eng'] == 'bar':
                bd = set(last_eng.values()) | set(last_chan.values())
                for en in ['pe', 'dve', 'act', 'pool', 'sp']:
                    pend[en] = set(pend.get(en, set())) | bd
                op['deps'] = set()
                continue
            if pend.get(op['eng']):
                deps |= pend[op['eng']]
                raw |= pend[op['eng']]
                pend[op['eng']] = set()
            if op['chan'] is not None:
                last_chan[op['chan']] = i
            else:
                last_eng[op['eng']] = i
            for t in op['r']:
                raw |= set(last_w.get(t, {}).values())
            for t in op['w']:
                deps |= set(last_w.get(t, {}).values())
                deps |= set(readers.get(t, {}).values())
            deps |= raw
            deps.discard(i)
            keep = set()
            for j in deps:
                pj = ops[j]
                if pj['chan'] is None and pj['eng'] == op['eng']:
                    if op['eng'] == 'pe':
                        continue
                    if j not in raw:
                        continue
                keep.add(j)
            op['deps'] = keep
            key = ('c', op['chan']) if op['chan'] is not None else ('e', op['eng'])
            for t in op['r']:
                readers.setdefault(t, {})[key] = i
            for t in op['w']:
                last_w.setdefault(t, {})[key] = i
        signal = set()
        for op in ops:
            for j in op['deps']:
                if ops[j]['chan'] is None:
                    signal.add(j)
        eng_cnt = {}
        chan_cnt = {}
        RING = {'sp': 16, 'pool': 8, 'act': 4}
        q_n = {}
        for i, op in enumerate(ops):
            if op['eng'] == 'bar':
                op['ev'] = None
                continue
            if op['chan'] is not None:
                q = op['eng']
                m = q_n.get(q, 0)
                q_n[q] = m + 1
                c = (q, m % RING[q])
                chan_cnt[c] = chan_cnt.get(c, 0) + 1
                op['ev'] = ('c', c, 16 * chan_cnt[c])
                op['ring_prev'] = 16 * (chan_cnt[c] - 1)
                op['ring'] = c
            elif i in signal:
                e = op['eng']
                eng_cnt[e] = eng_cnt.get(e, 0) + 1
                op['ev'] = ('e', e, eng_cnt[e])
            else:
                op['ev'] = None
        engs = ['pe', 'dve', 'act', 'pool', 'sp']
        stack = contextlib.ExitStack()
        sems = {}
        for e in engs:
            if e in eng_cnt:
                sems[('e', e)] = stack.enter_context(nc.semaphore('s_' + e))
        for c in chan_cnt:
            sems[('c', c)] = stack.enter_context(nc.semaphore('c_%d' % len(sems)))
        self.n_sems = len(sems)
        self.stats = (dict(eng_cnt), dict(chan_cnt), len(ops))
        by_eng = {e: [] for e in engs}
        for i, op in enumerate(ops):
            if op['eng'] != 'bar':
                by_eng[op['eng']].append(i)
        out_final = [(sems[('c', c)], 16 * chan_cnt[c]) for c in chan_cnt]

        def run_engine(ename, e):
            seen = {}
            for i in by_eng[ename]:
                op = ops[i]
                need = {}
                for j in op['deps']:
                    ev = ops[j]['ev']
                    k = (ev[0], ev[1])
                    need[k] = max(need.get(k, 0), ev[2])
                for k, v in need.items():
                    if seen.get(k, 0) >= v:
                        continue
                    e.wait_ge(sems[k], v)
                    seen[k] = v
                if op['chan'] is not None and op['ring_prev'] > 0:
                    k = ('c', op['ring'])
                    if seen.get(k, 0) < op['ring_prev']:
                        e.wait_ge(sems[k], op['ring_prev'])
                        seen[k] = op['ring_prev']
                ins = op['fn'](e)
                ev = op['ev']
                if op['chan'] is not None:
                    ins.then_inc(sems[('c', op['ring'])], 16)
                elif ev is not None:
                    ins.then_inc(sems[('e', ename)], 1)
            if ename == 'sp':
                for s, v in out_final:
                    e.wait_ge(s, v)

        with stack:
            with nc.Block() as block:
                @block.tensor
                def _(e):
                    run_engine('pe', e)

                @block.vector
                def _(e):
                    run_engine('dve', e)

                @block.scalar
                def _(e):
                    run_engine('act', e)

                @block.gpsimd
                def _(e):
                    run_engine('pool', e)

                @block.sync
                def _(e):
                    run_engine('sp', e)
        return nc


class WStream:
    def __init__(self, P, slots):
        self.P = P
        self.slots = slots
        self.tiles = []

    def get(self, src_ap, kt, ncols):
        i = len(self.tiles)
        s = i % NSLOT
        view = self.slots[s][:, 0:kt * ncols].rearrange("p (k n) -> p k n", k=kt)
        self.tiles.append(dict(src=src_ap, view=view, start=len(self.P.ops), end=None, slot=s))
        return i, view, ('wslot', s)

    def done(self, i):
        self.tiles[i]['end'] = len(self.P.ops)

    def finalize(self):
        P = self.P
        ins = []
        for i, t in enumerate(self.tiles):
            pos = 0 if i < NSLOT else self.tiles[i - NSLOT]['end']
            op = dict(eng='pool',
                      fn=(lambda e, o=t['view'], s=t['src']: e.dma_start(out=o, in_=s)),
                      r=(), w=(('wslot', t['slot']),), chan=('w', t['slot']))
            ins.append((pos, i, op))
        ins.sort(key=lambda x: (x[0], x[1]))
        new = []
        k = 0
        for idx in range(len(P.ops) + 1):
            while k < len(ins) and ins[k][0] == idx:
                new.append(ins[k][2])
                k += 1
            if idx < len(P.ops):
                new.append(P.ops[idx])
        P.ops = new


class B:
    pass


def build(debug=(), stop=None, dumps=None, nexp_dbg=NEXP, no_dg=False, rmode=0, rflags=(), lite=False):
    nc = bass.Bass("TRN2", target_bir_lowering=False)
    P = Prog(nc)
    st = contextlib.ExitStack()
    g = B()
    g.nexp_dbg = nexp_dbg
    g.no_dg = no_dg
    g.rmode = rmode
    g.rflags = set(rflags)
    DG_ap = []

    def din(name, shape):
        return nc.dram_tensor(name, list(shape), F32, kind="ExternalInput").ap()

    def dout(name, shape):
        return nc.dram_tensor(name, list(shape), F32, kind="ExternalOutput").ap()

    def dscr(name, shape):
        kind = "ExternalOutput" if name in debug else "Internal"
        return nc.dram_tensor(name, list(shape), F32, kind=kind).ap()

    xall = din("xall", [TA, D])
    call = din("call", [NSEQ, D])
    pmask_d = din("pmask", [128, 1])
    consts_d = din("consts", [128, NCONST])
    rows_d = din("rows", [NROW, D])
    convp_d = din("convp", [128, 32, 5])
    hp_d = din("hp", [128, 3, 32])
    wa2_d = din("gla_wa2", [16, 1024])
    sssm = din("sssm", [16, 2048, 128])
    sconv = din("sconv", [16, 3, 4096])
    sgla = din("sgla", [16, 1024, 512])
    ada_w = [din("ada_w0", [D, 6 * D]), din("ada_w1", [D, 6 * D])]
    ada_b = [din("ada_b0", [1, 6 * D]), din("ada_b1", [1, 6 * D])]
    w_in0 = din("w_in0", [D, NIN])
    w_out0 = din("w_out0", [2 * D, D])
    ffn_w1 = din("ffn_w1", [D, DFF])
    ffn_w3 = din("ffn_w3", [D, DFF])
    ffn_w2 = din("ffn_w2", [DFF, D])
    c_w_in = din("c_w_in", [D, 4 * D])
    c_w_out = din("c_w_out", [2 * D, D])
    wsT_d = din("wsT", [128, 8, 128])
    wsTs_d = din("wsTs", [64, 8, 64])
    cbs_d = din("cbs", [128, 8])
    cbss_d = din("cbss", [64, 8])
    router_d = din("router_w", [D, NEXP])
    nw = 0 if lite else NEXP
    moe_w1 = [din("moe_w1_%d" % i, [D, DFF]) for i in range(nw)]
    moe_w3 = [din("moe_w3_%d" % i, [D, DFF]) for i in range(nw)]
    moe_w2 = [din("moe_w2_%d" % i, [DFF, D]) for i in range(nw)]

    y_out = dout("y_out", [TM, D])
    ssm_p = dout("ssm_p", [2048, 128])
    conv_p = dout("conv_p", [3, 4096])
    gla_p = dout("gla_p", [1024, 512])
    ssm_s = dout("ssm_s", [16, 2048, 128])
    conv_s = dout("conv_s", [16, 3, 4096])
    gla_s = dout("gla_s", [16, 1024, 512])
    cmlp_v = dout("cmlp_v", [TS, 4096])

    mod_d = [dscr("mod0", [NSEQ, 6 * D]), dscr("mod1", [NSEQ, 6 * D])]
    xres = dscr("xres", [TM, D])
    proj = dscr("proj", [TA, NIN])
    xbcT = dscr("xbcT", [4096, TA])
    ymix = dscr("ymix", [TM, 2 * D])
    cu = dscr("cu", [TM, 4 * D])
    moe_acc = dscr("moe_acc", [TM, D])

    RA = st.enter_context(nc.sbuf_tensor("RA", [128, 23936], F32))
    RB = st.enter_context(nc.sbuf_tensor("RB", [128, 17408], F32))
    WSL = [st.enter_context(nc.sbuf_tensor("ws%d" % i, [128, SLOT_ELEMS], BF16)) for i in range(NSLOT)]
    CT = st.enter_context(nc.sbuf_tensor("CT", [128, NCONST], F32))
    PM = st.enter_context(nc.sbuf_tensor("PM", [128, 1], F32))
    HP = st.enter_context(nc.sbuf_tensor("HP", [128, 3, 32], F32))
    SMALL = st.enter_context(nc.sbuf_tensor("SMALL", [128, 1024], F32))
    PS = [st.enter_context(nc.psum_tensor("ps%d" % i, [128, 512], F32)) for i in range(8)]
    g.ps_i = 0
    g.uid = 0

    g.ps_pool = list(range(8))

    def nps():
        pool = g.ps_pool
        i = pool[g.ps_i % len(pool)]
        g.ps_i += 1
        return PS[i], ('ps', i)

    def uid(p='t'):
        g.uid += 1
        return (p, g.uid)

    def ra(off, n):
        return RA[:, off:off + n]

    def rb(off, n):
        return RB[:, off:off + n]

    W = WStream(P, WSL)

    def finish():
        W.finalize()
        with st:
            P.emit()
        nc._prog_stats = P.stats
        return nc

    def dump(name, ap, shape, dt=F32, r=()):
        if dumps is None or name not in dumps:
            return
        d = nc.dram_tensor(name, list(shape), dt, kind="ExternalOutput").ap()
        P.dma('sp', d, ap, r=list(r), chan=('dump', name), is_out=True)

    ident = CT[:, K_ID:K_ID + 128]
    ones_row = CT[0:1, K_ONE:K_ONE + 128]

    P.dma('sp', CT[:], consts_d, w=['CT'], chan='c0', mode_all=True)
    P.dma('sp', PM[:], pmask_d, w=['PM'], chan='c0', mode_all=True)
    P.dma('sp', HP[:], hp_d, w=['HP'], chan='c0', mode_all=True)

    def transpose_to(dst_fn, src, n, ncol, src_tok, dst_tok, eng_alt=[0]):
        nb = ncol // 128
        for j0 in range(0, nb, 4):
            nj = min(4, nb - j0)
            ps, pt = nps()
            for j in range(nj):
                P.pe(lambda e, ps=ps, j=j, j0=j0: e.transpose(ps[:, j * 128:j * 128 + n],
                                                              src[:n, (j0 + j) * 128:(j0 + j + 1) * 128],
                                                              ident[:n, :n]),
                     r=[src_tok, 'CT'], w=[pt])
            dst = dst_fn(j0, nj)
            srcv = ps[:, 0:nj * 128].rearrange("p (a b) -> p a b", a=nj)[:, :, 0:n]
            eng_alt[0] ^= 1
            if eng_alt[0]:
                P.act(lambda e, dst=dst, srcv=srcv: e.activation(out=dst, in_=srcv, func=AF.Copy),
                      r=[pt], w=[dst_tok])
            else:
                P.dve(lambda e, dst=dst, srcv=srcv: e.tensor_copy(out=dst, in_=srcv), r=[pt], w=[dst_tok])

    def bcast_rows_mm(dst, lhsT, rhs, n, ncol, r_toks, w_tok, evac):
        for c0 in range(0, ncol, 512):
            cn = min(512, ncol - c0)
            ps, pt = nps()
            P.pe(lambda e, ps=ps, c0=c0, cn=cn: e.matmul(ps[:n, :cn], lhsT, rhs[:, c0:c0 + cn],
                                                         start=True, stop=True),
                 r=list(r_toks), w=[pt])
            evac(ps[:n, :cn], dst[:n, c0:c0 + cn], pt, w_tok)

    cs = ra(0, 2048)
    scT = ra(2048, 16 * NSEQ // 2 + 8)
    scT = RA[:, 2048:2048 + 136].bitcast(BF16).rearrange("p (k s) -> p k s", k=16)
    brow = ra(4096, 12288)
    P.dma('sp', cs[:NSEQ, :], call, w=['cs'], chan='ld_a')
    P.act(lambda e: e.activation(out=cs[:NSEQ, :], in_=cs[:NSEQ, :], func=AF.Silu), r=['cs'], w=['cs'])
    transpose_to(lambda j0, nj: scT[:, j0:j0 + nj, :], cs, NSEQ, D, 'cs', 'scT')
    dump('d_cs', cs[:NSEQ, :], [NSEQ, D], F32, r=['cs'])
    dump('d_scT', scT, [128, 16, NSEQ], BF16, r=['scT'])
    ostg = [ra(16384 + i * 256, 256) for i in range(4)]
    for l in range(2):
        P.dma('sp', brow[0:1, :], ada_b[l], r=[], w=['brow'], chan='ld_a')
        for cb in range(48):
            c0 = cb * 256
            wi, wv, wt = W.get(ada_w[l][:, c0:c0 + 256].rearrange("(k p) n -> p k n", p=128), 16, 256)
            ps, pt = nps()
            for k in range(16):
                P.pe(lambda e, ps=ps, k=k, wv=wv: e.matmul(ps[:NSEQ, :256], scT[:, k, :], wv[:, k, :],
                                                            start=(k == 0), stop=False),
                     r=['scT', wt], w=[pt])
            P.pe(lambda e, ps=ps, c0=c0: e.matmul(ps[:NSEQ, :256], ones_row[:, :NSEQ], brow[0:1, c0:c0 + 256],
                                                  start=False, stop=True),
                 r=['CT', 'brow'], w=[pt])
            W.done(wi)
            sg = ostg[cb % 4]
            stok = ('ostg', cb % 4)
            P.dve(lambda e, sg=sg, ps=ps: e.tensor_copy(out=sg[:NSEQ, :], in_=ps[:NSEQ, :256]), r=[pt], w=[stok])
            P.dma('sp', mod_d[l][:, c0:c0 + 256], sg[:NSEQ, :], r=[stok], w=[('mod', l)], chan=('modst', cb % 4))


    if stop == 'ada':
        return finish()
    P.barrier()

    selP = CT[0:NSEQ, K_SELP:K_SELP + 128]
    selS = CT[0:NSEQ, K_SELS:K_SELS + 64]
    ALLT = [(i * 128, 128) for i in range(16)] + [(2048, 64)]
    MAINT = [(i * 128, 128) for i in range(8)] + [(1024, 64)]

    def make_mod(dst, l, j, n, sel, tmp, gam=None, gam_tok=None):
        tk = ('modtmp',)
        P.dma('sp', tmp[:NSEQ, :], mod_d[l][:, j * D:(j + 1) * D], r=[('mod', l)], w=[tk], chan='ld_m')
        dtok = ('modt', id(dst))
        for c0 in range(0, D, 512):
            ps, pt = nps()
            P.pe(lambda e, ps=ps, c0=c0: e.matmul(ps[:n, :512], sel, tmp[:NSEQ, c0:c0 + 512], start=True, stop=True),
                 r=[tk, 'CT'], w=[pt])
            if gam is not None:
                P.dve(lambda e, ps=ps, c0=c0: e.scalar_tensor_tensor(out=dst[:n, c0:c0 + 512], in0=ps[:n, :512], scalar=1.0,
                                                                      in1=gam[:n, c0:c0 + 512], op0=ALU.add, op1=ALU.mult),
                      r=[pt, gam_tok], w=[dtok])
            else:
                P.dve(lambda e, ps=ps, c0=c0: e.tensor_copy(out=dst[:n, c0:c0 + 512], in_=ps[:n, :512]), r=[pt], w=[dtok])
        return dtok

    def load_bc_row(dst, row, tok):
        P.dma('sp', dst, rows_d[row:row + 1, :].partition_broadcast(128), w=[tok], chan='ld_m')

    def norm_to_hT(src_rows, ttiles, hT, hT_tok, l, j_shift, j_scale, gamma_row, S, xbufs, with_mod=True, router=None, out_rows=None):
        gam = S['gam']
        gtok = ('gam',)
        load_bc_row(gam[:, :], gamma_row, gtok)
        if with_mod:
            s1p = make_mod(S['s1p'], l, j_scale, 128, selP, S['tmp'], gam, gtok)
            s2p = make_mod(S['s2p'], l, j_shift, 128, selP, S['tmp'])
            s1s = make_mod(S['s1s'], l, j_scale, 64, selS, S['tmp'], gam, gtok)
            s2s = make_mod(S['s2s'], l, j_shift, 64, selS, S['tmp'])
        def one_tile(ti, t0, n):
            xt = xbufs[ti % 2]
            xtok = ('xbuf', id(xbufs), ti % 2)
            P.dma('sp', xt[:n, :], src_rows(t0, n), r=[('xres',)], w=[xtok], chan=('ldx', ti % 2))
            stt = S['stat'][:, (ti % 2) * 4:(ti % 2) * 4 + 4]
            stok = ('stat', ti % 2)
            hb = S['h'][ti % 2]
            htok = ('hbuf', id(xbufs), ti % 2)
            P.act(lambda e, xt=xt, n=n, hb=hb: e.activation(out=hb[:n, :], in_=xt[:n, :], func=AF.Square,
                                                            accum_out=stt[:n, 0:1]),
                  r=[xtok], w=[stok, htok])
            P.act(lambda e, n=n: e.activation(out=stt[:n, 1:2], in_=stt[:n, 0:1], func=AF.Sqrt, scale=1.0 / D,
                                              bias=CT[:n, K_EPS:K_EPS + 1]),
                  r=[stok, 'CT'], w=[stok])
            P.dve(lambda e, n=n: e.reciprocal(out=stt[:n, 2:3], in_=stt[:n, 1:2]), r=[stok], w=[stok])
            if with_mod:
                is_s = (n == 64)
                s1, s1t = (S['s1s'], s1s) if is_s else (S['s1p'], s1p)
                s2, s2t = (S['s2s'], s2s) if is_s else (S['s2p'], s2p)
                P.dve(lambda e, xt=xt, n=n, hb=hb, s1=s1: e.scalar_tensor_tensor(out=hb[:n, :], in0=xt[:n, :], scalar=stt[:n, 2:3],
                                                                                 in1=s1[:n, :], op0=ALU.mult, op1=ALU.mult),
                      r=[xtok, stok, s1t], w=[htok])
                P.pool(lambda e, n=n, hb=hb, s2=s2: e.tensor_tensor(out=hb[:n, :], in0=hb[:n, :], in1=s2[:n, :], op=ALU.add),
                       r=[htok, s2t], w=[htok])
            else:
                P.dve(lambda e, xt=xt, n=n, hb=hb: e.scalar_tensor_tensor(out=hb[:n, :], in0=xt[:n, :], scalar=stt[:n, 2:3],
                                                                         in1=gam[:n, :], op0=ALU.mult, op1=ALU.mult),
                      r=[xtok, stok, gtok], w=[htok])
            if router is not None:
                router(ti, t0, n, hb, htok)
            elif hT is not None:
                transpose_to(lambda j0, nj, t0=t0, n=n: hT[:, j0:j0 + nj, t0:t0 + n], hb, n, D, htok, hT_tok)
            if out_rows is not None:
                P.dma('sp', out_rows(t0, n), hb[:n, :], r=[htok], w=[('yout',)], chan='x', is_out=True)
        for ti, (t0, n) in enumerate(ttiles):
            one_tile(ti, t0, n)

    def linear_A(hT, hT_tok, ttiles, Wap, K, col_blocks, evac):
        KT = K // 128
        for (c0, ncb) in col_blocks:
            wi, wv, wt = W.get(Wap[:, c0:c0 + ncb].rearrange("(k p) n -> p k n", p=128), KT, ncb)
            for ti, (t0, n) in enumerate(ttiles):
                ps, pt = nps()
                for k in range(KT):
                    P.pe(lambda e, ps=ps, k=k, t0=t0, n=n, wv=wv, ncb=ncb: e.matmul(
                        ps[:n, :ncb], hT[:, k, t0:t0 + n], wv[:, k, :], start=(k == 0), stop=(k == KT - 1)),
                        r=[hT_tok, wt], w=[pt])
                evac(ps, pt, ti, t0, n, c0, ncb)
            W.done(wi)

    STG = [SMALL[:, i * 256:(i + 1) * 256] for i in range(4)]
    g.stg_i = 0

    def stage_out(ps, pt, n, ncb, dst_ap, w_toks, scale_ap=None, is_out=False, accum=False, eng='dve'):
        i = g.stg_i % 4
        g.stg_i += 1
        sg = STG[i]
        stok = ('stg', i)
        if scale_ap is not None:
            P.dve(lambda e: e.tensor_scalar(out=sg[:n, :ncb], in0=ps[:n, :ncb], scalar1=scale_ap, scalar2=None, op0=ALU.mult),
                  r=[pt, 'PM'], w=[stok])
        elif eng == 'act':
            P.act(lambda e: e.activation(out=sg[:n, :ncb], in_=ps[:n, :ncb], func=AF.Copy), r=[pt], w=[stok])
        else:
            P.dve(lambda e: e.tensor_copy(out=sg[:n, :ncb], in_=ps[:n, :ncb]), r=[pt], w=[stok])
        P.dma('sp', dst_ap, sg[:n, :ncb], r=[stok], w=list(w_toks), chan=('stgc', i), is_out=is_out)

    hTall = RB[:, 0:16896].bitcast(BF16).rearrange("p (k t) -> p k t", k=16)
    S0 = dict(gam=ra(0, 2048), s1p=ra(2048, 2048), s2p=ra(4096, 2048), s1s=ra(6144, 2048), s2s=ra(8192, 2048),
              tmp=ra(10240, 2048), stat=ra(12288, 8), h=[ra(12544, 2048), ra(14592, 2048)])
    XB2 = [ra(16640, 2048), ra(18688, 2048)]
    P.dma('sp', xres, xall[TP:TA, :], w=[('xres',)], chan='ld_m')
    norm_to_hT(lambda t0, n: xall[t0:t0 + n, :], ALLT, hTall, 'hTall', 0, 0, 1, R_NM0, S0, XB2)
    dump('d_hT', hTall, [128, 16, TA], BF16, r=['hTall'])
    if stop == 'norm0':
        return finish()

    P.barrier()
    CVP = ra(0, 160).rearrange("p (c j) -> p c j", c=32)
    P.dma('sp', CVP, convp_d, w=['CVP'], chan='x')
    hst = ra(256, 4096)
    P.dma('sp', hst[:48, :], sconv.rearrange("b j c -> (b j) c"), w=['hst'], chan='x')
    histT = ra(4352, 32 * 48).rearrange("p (c r) -> p c r", c=32)
    transpose_to(lambda j0, nj: histT[:, j0:j0 + nj, :], hst, 48, 4096, 'hst', 'histT')
    PRE = [ra(6144, 2176), ra(8320, 2176)]
    PRS = [ra(10496, 112), ra(10608, 112)]
    ACC = [ra(10752, 2112), ra(12864, 2112)]
    csP = ra(14976, 96).rearrange("p (c j) -> p c j", c=32)
    csS = ra(15104, 32 * 48).rearrange("p (c r) -> p c r", c=32)
    for i in range(2):
        P.dve(lambda e, i=i: e.memset(PRE[i][:, 0:3], 0.0), w=[('pre', i)])
    TCH = [(0, 512), (512, 512), (1024, 512), (1536, 512), (2048, 64)]
    for cb in range(16):
        c0 = C_XBC + cb * 256
        wi, wv, wt = W.get(w_in0[:, c0:c0 + 256].rearrange("(k p) n -> p k n", p=128), 16, 256)
        for jj in range(2):
            ct = cb * 2 + jj
            b2 = ct % 2
            pre, prs, acc = PRE[b2], PRS[b2], ACC[b2]
            prs3 = prs.rearrange("p (b j) -> p b j", b=16)
            ptok, atok = ('pre', b2), ('acc', b2)
            for (t0, tn) in TCH:
                ps, pt = nps()
                for k in range(16):
                    P.pe(lambda e, ps=ps, k=k, t0=t0, tn=tn, wv=wv, jj=jj: e.matmul(
                        ps[:, :tn], wv[:, k, jj * 128:(jj + 1) * 128], hTall[:, k, t0:t0 + tn],
                        start=(k == 0), stop=(k == 15)), r=['hTall', wt], w=[pt])
                if t0 < TP:
                    P.dve(lambda e, ps=ps, t0=t0, tn=tn, pre=pre: e.tensor_scalar(
                        out=pre[:, 3 + t0:3 + t0 + tn], in0=ps[:, :tn], scalar1=PM[:, 0:1], scalar2=None, op0=ALU.mult),
                        r=[pt, 'PM'], w=[ptok])
                elif t0 < 2048:
                    P.act(lambda e, ps=ps, t0=t0, tn=tn, pre=pre: e.activation(out=pre[:, 3 + t0:3 + t0 + tn], in_=ps[:, :tn], func=AF.Copy),
                          r=[pt], w=[ptok])
                else:
                    P.act(lambda e, ps=ps, prs3=prs3: e.activation(out=prs3[:, :, 3:7], in_=ps[:, 0:64].rearrange("p (b j) -> p b j", b=16),
                                                                   func=AF.Copy), r=[pt], w=[ptok])
            P.dve(lambda e, prs3=prs3, ct=ct: e.tensor_copy(out=prs3[:, :, 0:3], in_=histT[:, ct, :].rearrange("p (b j) -> p b j", b=16)),
                  r=['histT'], w=[ptok])
            accs = acc[:, 2048:2112].rearrange("p (b j) -> p b j", b=16)
            P.act(lambda e, pre=pre, acc=acc, ct=ct: e.activation(out=acc[:, 0:2048], in_=pre[:, 3:2051], func=AF.Identity,
                                                                  scale=CVP[:, ct, 3:4], bias=CVP[:, ct, 4:5]),
                  r=[ptok, 'CVP'], w=[atok])
            P.act(lambda e, prs3=prs3, accs=accs, ct=ct: e.activation(out=accs, in_=prs3[:, :, 3:7], func=AF.Identity,
                                                                     scale=CVP[:, ct, 3:4], bias=CVP[:, ct, 4:5]),
                  r=[ptok, 'CVP'], w=[atok])
            for j in range(3):
                P.dve(lambda e, pre=pre, acc=acc, ct=ct, j=j: e.scalar_tensor_tensor(
                    out=acc[:, 0:2048], in0=pre[:, j:j + 2048], scalar=CVP[:, ct, j:j + 1], in1=acc[:, 0:2048],
                    op0=ALU.mult, op1=ALU.add), r=[ptok, atok, 'CVP'], w=[atok])
                P.dve(lambda e, prs3=prs3, accs=accs, ct=ct, j=j: e.scalar_tensor_tensor(
                    out=accs, in0=prs3[:, :, j:j + 4], scalar=CVP[:, ct, j:j + 1], in1=accs,
                    op0=ALU.mult, op1=ALU.add), r=[ptok, atok, 'CVP'], w=[atok])
            P.act(lambda e, acc=acc: e.activation(out=acc[:, :], in_=acc[:, :], func=AF.Silu), r=[atok], w=[atok])
            P.dma('sp', xbcT[ct * 128:(ct + 1) * 128, :], acc[:, :], r=[atok], w=[('xbcT',)], chan='x')
            P.dve(lambda e, pre=pre, ct=ct: e.tensor_copy(out=csP[:, ct, :], in_=pre[:, 2048:2051]), r=[ptok], w=['csP'])
            P.dve(lambda e, prs3=prs3, ct=ct: e.tensor_copy(out=csS[:, ct, :].rearrange("p (b j) -> p b j", b=16), in_=prs3[:, :, 4:7]),
                  r=[ptok], w=['csS'])
        W.done(wi)
    cso = ra(16640, 4096)
    for (src3, nr, dst, tok) in [(csP, 3, conv_p, 'csP'), (csS, 48, conv_s.rearrange("b j c -> (b j) c"), 'csS')]:
        for j0 in range(0, 32, 4):
            ps, pt = nps()
            for j in range(4):
                P.pe(lambda e, ps=ps, j=j, j0=j0, src3=src3, nr=nr: e.transpose(ps[:nr, j * 128:(j + 1) * 128], src3[:, j0 + j, :], ident),
                     r=[tok, 'CT'], w=[pt])
            P.dve(lambda e, ps=ps, j0=j0, nr=nr: e.tensor_copy(out=cso[:nr, j0 * 128:(j0 + 4) * 128], in_=ps[:nr, :]), r=[pt], w=['cso'])
        P.dma('sp', dst, cso[:nr, :], r=['cso'], w=[('convout', tok)], chan='x', is_out=True)
    dump('d_xbcT', None, None)
    if stop == 'conv':
        return finish()

    cols_all = [(C_DT, 32)] + [(c, 256) for c in range(C_K, C_R, 256)] + [(C_G, 16)]
    cols_main = [(c, 256) for c in range(0, 2048, 256)] + [(c, 256) for c in range(C_Q, C_K, 256)]
    cols_main += [(c, 256) for c in range(C_R, C_G, 256)]

    def evac_proj(ps, pt, ti, t0, n, c0, ncb):
        stage_out(ps, pt, n, ncb, proj[t0:t0 + n, c0:c0 + ncb], [('proj',)],
                  scale_ap=(PM[:n, 0:1] if t0 < TP else None), eng=('act' if ti % 2 else 'dve'))
    linear_A(hTall, 'hTall', ALLT, w_in0, D, cols_all, evac_proj)
    linear_A(hTall, 'hTall', ALLT[8:], w_in0, D, cols_main, evac_proj)
    if stop == 'proj':
        return finish()

    P.barrier()
    g.ps_pool = [0, 1, 2, 3]
    XB = ra(0, 4096).rearrange("p (c t) -> p c t", c=32)
    xs = ra(4096, 2048)
    bm = ra(6144, 1024)
    xdt = ra(7168, 2048)
    xdte = ra(9216, 2048)
    ya = ra(11264, 2048)
    zt = ra(13312, 2048)
    HT = ra(15360, 2048)
    sm = ra(17408, 256)
    dtp, dt_, da, acum, eA, toend, cdv, dif = [sm[:, i * 32:(i + 1) * 32] for i in range(8)]
    CBm = [ra(17664, 128), ra(17792, 128)]
    LhB = [ra(21824, 512), ra(22336, 512)]
    EbB = [ra(22848, 512), ra(23360, 512)]
    tmpy = ra(18688, 512)
    tmp2 = ra(19200, 512)
    yoacc = ra(19712, 2048)
    stat = ra(21760, 16)
    ANEG = ra(21776, 32)
    DSK = HP[:, 2, :]
    h0 = ra(21824, 2048).rearrange("p (j n) -> p j n", j=16)
    SG = rb(0, 4096).rearrange("p (a v) -> p a v", a=8)
    qk = rb(4096, 2048)
    vv = rb(6144, 2048)
    rr = rb(8192, 2048)
    glr = rb(10240, 16)
    glrT = rb(10256, 64)
    GB = [dict(la=rb(10320, 256), bcs=rb(10576, 256), q_t=rb(10832, 256), k_t=rb(11088, 256), k_e=rb(11344, 256),
               qtT=rb(11600, 128).rearrange("p (k t) -> p k t", k=2), ktT=rb(11728, 128).rearrange("p (k t) -> p k t", k=2),
               attm=rb(11856, 64), decT=rb(11920, 32)),
          dict(la=ra(17920, 256), bcs=ra(18176, 256), q_t=ra(18432, 256), k_t=ra(20736, 256), k_e=ra(20992, 256),
               qtT=ra(21248, 128).rearrange("p (k t) -> p k t", k=2), ktT=ra(21376, 128).rearrange("p (k t) -> p k t", k=2),
               attm=ra(21504, 64), decT=ra(21568, 32))]
    yb = rb(11952, 2048)
    WA2 = rb(14000, 1024)
    BAr = rb(15024, 1024)
    h0T = rb(4096, 2048)
    sstg = rb(6144, 2048)
    xdtem = rb(8192, 2048)
    cdq = rb(16048, 256).rearrange("p (j b) -> p j b", j=16)
    cdexp = rb(16304, 1024)
    S0b = rb(16304, 1024).rearrange("p (k v) -> p k v", k=2)
    P.dma('sp', WA2[:16, :], wa2_d, w=['WA2'], chan='x')
    P.dma('sp', BAr[0:1, :], rows_d[R_BA:R_BA + 1, 0:1024], w=['BAr'], chan='x')
    P.act(lambda e: e.activation(out=ANEG[:, :], in_=HP[:, 1, :], func=AF.Exp), r=['HP'], w=['ANEG'])
    P.dve(lambda e: e.tensor_scalar(out=ANEG[:, :], in0=ANEG[:, :], scalar1=-1.0, scalar2=None, op0=ALU.mult), r=['ANEG'], w=['ANEG'])
    P.dve(lambda e: e.memset(HT[:, :], 0.0), w=[('HT', q) for q in range(8)])
    P.dve(lambda e: e.memset(RB[:, 0:4096], 0.0), w=[('SG', q) for q in range(4)])
    ONEC = CT[:, K_ONE:K_ONE + 1]
    EPSC = CT[:, K_EPS:K_EPS + 1]
    SEQM = CT[:, K_SEQM:K_SEQM + 16]
    PICK = CT[:, K_PICK:K_PICK + 16]

    def rstd_from(ssq_ap, out_ap, n, width, toks):
        P.act(lambda e: e.activation(out=out_ap, in_=ssq_ap, func=AF.Sqrt, scale=1.0 / width, bias=EPSC[:n, :]), r=toks + ['CT'], w=toks)
        P.dve(lambda e: e.reciprocal(out=out_ap, in_=out_ap), r=toks, w=toks)

    def ssd_tile(t0, n, mode):
        smp = (mode == 'sample')
        g.ps_pool = [0, 1, 2, 3]
        Lm = CT[:n, (K_LS if smp else K_L):(K_LS if smp else K_L) + n]
        Rm = CT[:n, (K_RS if smp else K_R):(K_RS if smp else K_R) + n]
        CMm = CT[:n, (K_CMS if smp else K_CM):(K_CMS if smp else K_CM) + n]
        BLm = CT[:n, (K_BLKS if smp else K_BLK):(K_BLKS if smp else K_BLK) + n]
        P.dma('sp', XB[:, :, :n], xbcT[:, t0:t0 + n].rearrange("(c p) t -> p c t", p=128), r=[('xbcT',)], w=['XB'], chan='x')
        P.dma('sp', dtp[:n, :], proj[t0:t0 + n, C_DT:C_DT + 32], r=[('proj',)], w=['sm'], chan='x')
        if mode != 'prefix':
            P.dma('sp', zt[:n, :], proj[t0:t0 + n, 0:2048], r=[('proj',)], w=['zt'], chan='x')
        P.dve(lambda e: e.tensor_tensor(out=dt_[:n, :], in0=dtp[:n, :], in1=HP[:n, 0, :], op=ALU.add), r=['sm', 'HP'], w=['sm'])
        P.act(lambda e: e.activation(out=dt_[:n, :], in_=dt_[:n, :], func=AF.Exp), r=['sm'], w=['sm'])
        P.act(lambda e: e.activation(out=dt_[:n, :], in_=dt_[:n, :], func=AF.Ln, bias=ONEC[:n, :]), r=['sm', 'CT'], w=['sm'])
        P.dve(lambda e: e.tensor_tensor(out=da[:n, :], in0=dt_[:n, :], in1=ANEG[:n, :], op=ALU.mult), r=['sm', 'ANEG'], w=['sm'])
        psA, ptA = nps()
        P.pe(lambda e: e.matmul(psA[:n, 0:32], Rm, da[:n, :], start=True, stop=True), r=['sm', 'CT'], w=[ptA])
        P.pe(lambda e: e.matmul(psA[:n, 32:64], BLm, da[:n, :], start=True, stop=True), r=['sm', 'CT'], w=[ptA])
        P.dve(lambda e: e.tensor_copy(out=acum[:n, :], in_=psA[:n, 0:32]), r=[ptA], w=['sm'])
        P.act(lambda e: e.activation(out=eA[:n, :], in_=psA[:n, 0:32], func=AF.Exp), r=[ptA], w=['sm'])
        P.act(lambda e: e.activation(out=cdv[:n, :], in_=psA[:n, 32:64], func=AF.Exp), r=[ptA], w=['sm'])
        P.dve(lambda e: e.tensor_tensor(out=dif[:n, :], in0=psA[:n, 32:64], in1=acum[:n, :], op=ALU.subtract), r=[ptA, 'sm'], w=['sm'])
        P.act(lambda e: e.activation(out=toend[:n, :], in_=dif[:n, :], func=AF.Exp), r=['sm'], w=['sm'])
        for (dst, cbase, nblk, tok) in [(xs, 0, 16, 'xs'), (bm, 16, 8, 'bm')]:
            for j0 in range(0, nblk, 4):
                ps, pt = nps()
                for j in range(4):
                    P.pe(lambda e, ps=ps, j=j, j0=j0, cbase=cbase: e.transpose(ps[:n, j * 128:(j + 1) * 128], XB[:, cbase + j0 + j, :n], ident),
                         r=['XB', 'CT'], w=[pt])
                P.act(lambda e, ps=ps, j0=j0, dst=dst: e.activation(out=dst[:n, j0 * 128:(j0 + 4) * 128], in_=ps[:n, :], func=AF.Copy), r=[pt], w=[tok])
        x3 = lambda ap, c0=0, nh=32: ap[:n, c0 * 64:(c0 + nh) * 64].rearrange("p (h d) -> p h d", h=nh)
        bc3 = lambda ap, c0=0, nh=32: ap[:n, c0:c0 + nh].unsqueeze(2).to_broadcast([n, nh, 64])
        P.dve(lambda e: e.tensor_tensor(out=x3(xdt), in0=x3(xs), in1=bc3(dt_), op=ALU.mult), r=['xs', 'sm'], w=['xdt'])
        P.pool(lambda e: e.tensor_tensor(out=x3(xdte), in0=x3(xdt), in1=bc3(toend), op=ALU.mult), r=['xdt', 'sm'], w=['xdte'])
        if smp:
            ssd_sample_states(n)
        if mode != 'prefix':
            for gp in range(4):
                psy = PS[4 + 2 * (gp % 2)]
                pty = ('ps', 4 + 2 * (gp % 2))
                pso = PS[5 + 2 * (gp % 2)]
                pto = ('ps', 5 + 2 * (gp % 2))
                for gi in range(2):
                    gq = 2 * gp + gi
                    cb = CBm[gq % 2]
                    ps, pt = nps()
                    P.pe(lambda e, ps=ps, gq=gq: e.matmul(ps[:n, :n], XB[:, 16 + gq, :n], XB[:, 24 + gq, :n], start=True, stop=True), r=['XB'], w=[pt])
                    P.dve(lambda e, ps=ps, cb=cb: e.tensor_tensor(out=cb[:n, :n], in0=ps[:n, :n], in1=CMm, op=ALU.mult), r=[pt, 'CT'], w=[('CBm', gq % 2)])
                    b2 = gq % 2
                    L4 = LhB[b2][:n, 0:4 * n].rearrange("p (h t) -> p h t", h=4)
                    E4 = EbB[b2][:n, 0:4 * n].rearrange("p (h t) -> p h t", h=4)
                    h0w = ['h0'] if smp else []
                    P.dve(lambda e, L4=L4, gq=gq: e.tensor_tensor(out=L4, in0=Lm.unsqueeze(1).to_broadcast([n, 4, n]),
                                                                   in1=da[:n, 4 * gq:4 * gq + 4].unsqueeze(2).to_broadcast([n, 4, n]), op=ALU.mult),
                          r=['sm', 'CT'], w=[('Lh', b2)] + h0w)
                    ps, pt = nps()
                    for hh in range(4):
                        P.pe(lambda e, ps=ps, b2=b2, hh=hh: e.matmul(ps[:n, hh * n:(hh + 1) * n], LhB[b2][:n, hh * n:(hh + 1) * n], Rm, start=True, stop=True),
                             r=[('Lh', b2), 'CT'], w=[pt])
                    P.act(lambda e, ps=ps, b2=b2: e.activation(out=EbB[b2][:n, 0:4 * n], in_=ps[:n, 0:4 * n], func=AF.Exp), r=[pt], w=[('Eb', b2)] + h0w)
                    P.pool(lambda e, E4=E4, cb=cb: e.tensor_tensor(out=E4, in0=E4, in1=cb[:n, :n].unsqueeze(1).to_broadcast([n, 4, n]), op=ALU.mult),
                           r=[('Eb', b2), ('CBm', gq % 2)], w=[('Eb', b2)])
                    for hh in range(4):
                        h = 4 * gq + hh
                        hc = (h % 8) * 64
                        P.pe(lambda e, b2=b2, h=h, hh=hh, hc=hc, psy=psy: e.matmul(psy[:n, hc:hc + 64], EbB[b2][:n, hh * n:(hh + 1) * n], xdt[:n, h * 64:(h + 1) * 64], start=True, stop=True),
                             r=[('Eb', b2), 'xdt'], w=[pty])
                    if not smp:
                        P.pe(lambda e, gq=gq, gi=gi, pso=pso: e.matmul(pso[:n, gi * 256:(gi + 1) * 256], XB[:, 24 + gq, :n], HT[:, gq * 256:(gq + 1) * 256], start=True, stop=True),
                             r=['XB', ('HT', gq)], w=[pto])
                yo_src = yoacc[:n, gp * 512:(gp + 1) * 512] if smp else pso[:n, :]
                yo_tok = 'yoacc' if smp else pto
                v8 = lambda ap: ap.rearrange("p (h d) -> p h d", h=8)
                P.dve(lambda e, yo_src=yo_src, gp=gp: e.tensor_tensor(out=v8(tmpy[:n, :]), in0=v8(yo_src), in1=bc3(eA, 8 * gp, 8), op=ALU.mult),
                      r=[yo_tok, 'sm'], w=['tmpy'])
                P.dve(lambda e, psy=psy: e.tensor_tensor(out=tmpy[:n, :], in0=tmpy[:n, :], in1=psy[:n, :], op=ALU.add), r=['tmpy', pty], w=['tmpy'])
                P.pool(lambda e, gp=gp: e.tensor_tensor(out=v8(tmp2[:n, :]), in0=x3(xs, 8 * gp, 8), in1=DSK[:n, 8 * gp:8 * gp + 8].unsqueeze(2).to_broadcast([n, 8, 64]), op=ALU.mult),
                       r=['xs', 'HP'], w=['tmp2'])
                P.pool(lambda e, gp=gp: e.tensor_tensor(out=ya[:n, gp * 512:(gp + 1) * 512], in0=tmpy[:n, :], in1=tmp2[:n, :], op=ALU.add), r=['tmpy', 'tmp2'], w=['ya'])
        if not smp:
            for gq in range(8):
                ps, pt = nps()
                P.pe(lambda e, ps=ps, gq=gq: e.matmul(ps[:, 0:256], bm[:n, gq * 128:(gq + 1) * 128], xdte[:n, gq * 256:(gq + 1) * 256], start=True, stop=True),
                     r=['bm', 'xdte'], w=[pt])
                hv = HT[:, gq * 256:(gq + 1) * 256].rearrange("p (h d) -> p h d", h=4)
                P.dve(lambda e, hv=hv, gq=gq: e.tensor_tensor(out=hv, in0=hv, in1=cdv[:, 4 * gq:4 * gq + 4].unsqueeze(2).to_broadcast([128, 4, 64]), op=ALU.mult),
                      r=[('HT', gq), 'sm'], w=[('HT', gq)])
                P.dve(lambda e, ps=ps, gq=gq: e.tensor_tensor(out=HT[:, gq * 256:(gq + 1) * 256], in0=HT[:, gq * 256:(gq + 1) * 256], in1=ps[:, 0:256], op=ALU.add),
                      r=[('HT', gq), pt], w=[('HT', gq)])
        if mode != 'prefix':
            P.act(lambda e: e.activation(out=zt[:n, :], in_=zt[:n, :], func=AF.Silu), r=['zt'], w=['zt'])
            P.dve(lambda e: e.tensor_tensor(out=ya[:n, :], in0=ya[:n, :], in1=zt[:n, :], op=ALU.mult), r=['ya', 'zt'], w=['ya'])
            P.act(lambda e: e.activation(out=zt[:n, :], in_=ya[:n, :], func=AF.Square, accum_out=stat[:n, 0:1]), r=['ya'], w=['zt', 'stat'])
            rstd_from(stat[:n, 0:1], stat[:n, 1:2], n, 2048.0, ['stat'])
            P.dve(lambda e: e.tensor_scalar(out=ya[:n, :], in0=ya[:n, :], scalar1=stat[:n, 1:2], scalar2=None, op0=ALU.mult),
                  r=['ya', 'stat'], w=['ya'])
            P.dma('sp', ymix[t0 - TP:t0 - TP + n, 0:2048], ya[:n, :], r=['ya'], w=[('ymix',)], chan='x')

    def ssd_sample_states(n):
        cdx = yoacc
        P.dve(lambda e: e.tensor_copy(out=cdx[:n, :].rearrange("p (h d) -> p h d", h=32), in_=cdv[:n, :].unsqueeze(2).to_broadcast([n, 32, 64])), r=['sm'], w=['yoacc'])
        ps, pt = nps()
        for j in range(16):
            P.pe(lambda e, ps=ps, j=j: e.matmul(ps[:, j * 16:(j + 1) * 16], cdx[:n, j * 128:(j + 1) * 128], PICK[:n, :], start=True, stop=True), r=['yoacc', 'CT'], w=[pt])
        P.dve(lambda e, ps=ps: e.tensor_copy(out=cdq, in_=ps[:, 0:256].rearrange("p (j b) -> p j b", j=16)), r=[pt], w=['cdq'])
        for b in range(16):
            P.dma('sp', h0, sssm[b].rearrange("(j q) n -> q j n", q=128), w=['h0'], chan='x')
            for j0 in range(0, 16, 4):
                ps, pt = nps()
                for j in range(4):
                    P.pe(lambda e, ps=ps, j=j, j0=j0: e.transpose(ps[:, j * 128:(j + 1) * 128], h0[:, j0 + j, :], ident), r=['h0', 'CT'], w=[pt])
                P.act(lambda e, ps=ps, j0=j0: e.activation(out=h0T[:, j0 * 128:(j0 + 4) * 128], in_=ps[:, :], func=AF.Copy), r=[pt], w=['h0T'])
            for gp in range(4):
                ps, pt = nps()
                for gi in range(2):
                    gq = 2 * gp + gi
                    P.pe(lambda e, ps=ps, gq=gq, gi=gi: e.matmul(ps[:n, gi * 256:(gi + 1) * 256], XB[:, 24 + gq, :n], h0T[:, gq * 256:(gq + 1) * 256], start=True, stop=True),
                         r=['XB', 'h0T'], w=[pt])
                dst = yoacc[:n, gp * 512:(gp + 1) * 512]
                if b == 0:
                    P.dve(lambda e, ps=ps, dst=dst, b=b: e.tensor_scalar(out=dst, in0=ps[:n, :], scalar1=SEQM[:n, b:b + 1], scalar2=None, op0=ALU.mult), r=[pt, 'CT', 'cdq'], w=['yoacc'])
                else:
                    P.dve(lambda e, ps=ps, dst=dst, b=b: e.scalar_tensor_tensor(out=dst, in0=ps[:n, :], scalar=SEQM[:n, b:b + 1], in1=dst, op0=ALU.mult, op1=ALU.add),
                          r=[pt, 'CT', 'yoacc'], w=['yoacc'])
            P.dve(lambda e, b=b: e.tensor_scalar(out=xdtem[:n, :], in0=xdte[:n, :], scalar1=SEQM[:n, b:b + 1], scalar2=None, op0=ALU.mult), r=['xdte', 'CT'], w=['xdtem'])
            for j0 in range(0, 16, 4):
                ps, pt = nps()
                for j in range(4):
                    jj = j0 + j
                    P.pe(lambda e, ps=ps, j=j, jj=jj: e.matmul(ps[:, j * 128:(j + 1) * 128], xdtem[:n, jj * 128:(jj + 1) * 128], bm[:n, (jj // 2) * 128:(jj // 2 + 1) * 128], start=True, stop=True),
                         r=['xdtem', 'bm'], w=[pt])
                sv = sstg[:, j0 * 128:(j0 + 4) * 128].rearrange("p (j n) -> p j n", j=4)
                P.dve(lambda e, sv=sv, j0=j0, b=b: e.tensor_tensor(out=sv, in0=h0[:, j0:j0 + 4, :], in1=cdq[:, j0:j0 + 4, b:b + 1].to_broadcast([128, 4, 128]), op=ALU.mult),
                      r=['h0', 'cdq'], w=['sstg'])
                P.dve(lambda e, ps=ps, j0=j0: e.tensor_tensor(out=sstg[:, j0 * 128:(j0 + 4) * 128], in0=sstg[:, j0 * 128:(j0 + 4) * 128], in1=ps[:, :], op=ALU.add), r=['sstg', pt], w=['sstg'])
            P.dma('sp', ssm_s[b].rearrange("(j q) n -> q j n", q=128), sstg.rearrange("p (j n) -> p j n", j=16), r=['sstg'], w=[('ssm_s',)], chan='x', is_out=True)

    def gla_chunk(t0, n, mode):
        smp = (mode == 'sample')
        g.ps_pool = [0, 1, 2, 3, 6, 7]
        Rm = CT[:n, (K_RS if smp else K_R):(K_RS if smp else K_R) + n]
        CMm = CT[:n, (K_CMS if smp else K_CM):(K_CMS if smp else K_CM) + n]
        BLm = CT[:n, (K_BLKS if smp else K_BLK):(K_BLKS if smp else K_BLK) + n]
        if mode == 'prefix':
            P.dma('sp', qk[:n, 1024:2048], proj[t0:t0 + n, C_K:C_K + 1024], r=[('proj',)], w=['qk'], chan='x')
        else:
            P.dma('sp', qk[:n, :], proj[t0:t0 + n, C_Q:C_Q + 2048], r=[('proj',)], w=['qk'], chan='x')
        P.dma('sp', vv[:n, :], proj[t0:t0 + n, C_V:C_V + 2048], r=[('proj',)], w=['vv'], chan='x')
        P.dma('sp', glr[:n, :], proj[t0:t0 + n, C_G:C_G + 16], r=[('proj',)], w=['glr'], chan='x')
        if mode != 'prefix':
            P.dma('sp', rr[:n, :], proj[t0:t0 + n, C_R:C_R + 2048], r=[('proj',)], w=['rr'], chan='x')
            P.act(lambda e: e.activation(out=rr[:n, :], in_=rr[:n, :], func=AF.Silu), r=['rr'], w=['rr'])
        ps, pt = nps()
        P.pe(lambda e, ps=ps: e.transpose(ps[:16, 0:n], glr[:n, :], ident[:n, :n]), r=['glr', 'CT'], w=[pt])
        P.dve(lambda e, ps=ps: e.tensor_copy(out=glrT[:16, :n], in_=ps[:16, 0:n]), r=[pt], w=['glrT'])
        def head(hd):
            hb2 = hd % 2
            G_ = GB[hb2]
            la, bcs, q_t, k_t, k_e, qtT, ktT, attm, decT = (G_[k_] for k_ in 'la bcs q_t k_t k_e qtT ktT attm decT'.split())
            ps, pt = nps()
            P.pe(lambda e, ps=ps, hd=hd: e.matmul(ps[:n, 0:256], glrT[:16, :n], WA2[:16, hd * 256:(hd + 1) * 256], start=True, stop=False), r=['glrT', 'WA2'], w=[pt])
            P.pe(lambda e, ps=ps, hd=hd: e.matmul(ps[:n, 0:256], ones_row[:, :n], BAr[0:1, hd * 256:(hd + 1) * 256], start=False, stop=True), r=['CT', 'BAr'], w=[pt])
            P.act(lambda e, ps=ps: e.activation(out=la[:n, :], in_=ps[:n, 0:256], func=AF.Exp, scale=-1.0), r=[pt], w=[('la', hb2)])
            P.act(lambda e: e.activation(out=la[:n, :], in_=la[:n, :], func=AF.Ln, bias=ONEC[:n, :]), r=[('la', hb2), 'CT'], w=[('la', hb2)])
            ps, pt = nps()
            P.pe(lambda e, ps=ps: e.matmul(ps[:n, 0:256], Rm, la[:n, :], start=True, stop=True), r=[('la', hb2), 'CT'], w=[pt])
            P.pe(lambda e, ps=ps: e.matmul(ps[:n, 256:512], BLm, la[:n, :], start=True, stop=True), r=[('la', hb2), 'CT'], w=[pt])
            ps2, pt2 = nps()
            ncol = 16 if smp else 1
            rhsm = SEQM[:n, :] if smp else ONEC[:n, :]
            for kt in range(2):
                P.pe(lambda e, ps2=ps2, kt=kt: e.matmul(ps2[:, kt * 16:kt * 16 + ncol], la[:n, kt * 128:(kt + 1) * 128], rhsm, start=True, stop=True), r=[('la', hb2), 'CT'], w=[pt2])
            P.act(lambda e, ps2=ps2: e.activation(out=decT[:, :], in_=ps2[:, 0:32], func=AF.Exp, scale=-1.0 / 16.0), r=[pt2], w=[('decT', hb2)])
            P.dve(lambda e, ps=ps: e.tensor_copy(out=bcs[:n, :], in_=ps[:n, 0:256]), r=[pt], w=[('bcs', hb2)])
            qh = qk[:n, hd * 256:(hd + 1) * 256]
            kh = qk[:n, 1024 + hd * 256:1024 + (hd + 1) * 256]
            P.act(lambda e, ps=ps: e.activation(out=k_t[:n, :], in_=ps[:n, 0:256], func=AF.Exp, scale=1.0 / 16.0), r=[pt], w=[('k_t', hb2)])
            P.dve(lambda e, kh=kh: e.tensor_tensor(out=k_t[:n, :], in0=k_t[:n, :], in1=kh, op=ALU.mult), r=[('k_t', hb2), 'qk'], w=[('k_t', hb2)])
            P.dve(lambda e, ps=ps: e.tensor_tensor(out=k_e[:n, :], in0=bcs[:n, :], in1=ps[:n, 256:512], op=ALU.subtract), r=[('bcs', hb2), pt], w=[('k_e', hb2)])
            P.act(lambda e: e.activation(out=k_e[:n, :], in_=k_e[:n, :], func=AF.Exp, scale=1.0 / 16.0), r=[('k_e', hb2)], w=[('k_e', hb2)])
            P.dve(lambda e, kh=kh: e.tensor_tensor(out=k_e[:n, :], in0=k_e[:n, :], in1=kh, op=ALU.mult), r=[('k_e', hb2), 'qk'], w=[('k_e', hb2)])
            if mode != 'prefix':
                P.act(lambda e, ps=ps: e.activation(out=q_t[:n, :], in_=ps[:n, 0:256], func=AF.Exp, scale=-1.0 / 16.0), r=[pt], w=[('q_t', hb2)])
                P.dve(lambda e, qh=qh: e.scalar_tensor_tensor(out=q_t[:n, :], in0=qh, scalar=0.0625, in1=q_t[:n, :], op0=ALU.mult, op1=ALU.mult), r=[('q_t', hb2), 'qk'], w=[('q_t', hb2)])
                for (src, dstT, tk, tkT) in [(q_t, qtT, ('q_t', hb2), ('qtT', hb2)), (k_t, ktT, ('k_t', hb2), ('ktT', hb2))]:
                    ps3, pt3 = nps()
                    for kt in range(2):
                        P.pe(lambda e, ps3=ps3, kt=kt, src=src: e.transpose(ps3[:, kt * 64:kt * 64 + n], src[:n, kt * 128:(kt + 1) * 128], ident[:n, :n]), r=[tk, 'CT'], w=[pt3])
                    P.dve(lambda e, ps3=ps3, dstT=dstT: e.tensor_copy(out=dstT[:, :, :n], in_=ps3[:, 0:128].rearrange("p (k t) -> p k t", k=2)[:, :, :n]), r=[pt3], w=[tkT])
                ps4, pt4 = nps()
                for kt in range(2):
                    P.pe(lambda e, ps4=ps4, kt=kt: e.matmul(ps4[:n, :n], ktT[:, kt, :n], qtT[:, kt, :n], start=(kt == 0), stop=(kt == 1)), r=[('ktT', hb2), ('qtT', hb2)], w=[pt4])
                P.dve(lambda e, ps4=ps4: e.tensor_tensor(out=attm[:n, :n], in0=ps4[:n, :n], in1=CMm, op=ALU.mult), r=[pt4, 'CT'], w=[('attm', hb2)])
                pso = PS[4 + hd % 2]
                pto = ('ps', 4 + hd % 2)
                vh = vv[:n, hd * 512:(hd + 1) * 512]
                if not smp:
                    P.pe(lambda e, pso=pso, vh=vh: e.matmul(pso[:n, :], attm[:n, :n], vh, start=True, stop=False), r=[('attm', hb2), 'vv'], w=[pto])
                    for kt in range(2):
                        P.pe(lambda e, pso=pso, kt=kt, hd=hd: e.matmul(pso[:n, :], qtT[:, kt, :n], SG[:, hd * 2 + kt, :], start=False, stop=(kt == 1)), r=[('qtT', hb2), ('SG', hd)], w=[pto])
                    o_src, o_tok = pso[:n, :], pto
                else:
                    P.pe(lambda e, pso=pso, vh=vh: e.matmul(pso[:n, :], attm[:n, :n], vh, start=True, stop=True), r=[('attm', hb2), 'vv'], w=[pto])
                    P.dve(lambda e, pso=pso: e.tensor_copy(out=tmp2[:n, :], in_=pso[:n, :]), r=[pto], w=['tmp2'])
                    for b in range(16):
                        P.dma('sp', S0b, sgla[b, hd * 256:(hd + 1) * 256, :].rearrange("(k p) v -> p k v", p=128), w=['S0b'], chan='x')
                        ps5, pt5 = nps()
                        for kt in range(2):
                            P.pe(lambda e, ps5=ps5, kt=kt: e.matmul(ps5[:n, :], qtT[:, kt, :n], S0b[:, kt, :], start=(kt == 0), stop=(kt == 1)), r=[('qtT', hb2), 'S0b'], w=[pt5])
                        P.dve(lambda e, ps5=ps5, b=b: e.scalar_tensor_tensor(out=tmp2[:n, :], in0=ps5[:n, :], scalar=SEQM[:n, b:b + 1], in1=tmp2[:n, :], op0=ALU.mult, op1=ALU.add),
                              r=[pt5, 'tmp2', 'CT'], w=['tmp2'])
                        P.dve(lambda e, b=b, vh=vh: e.tensor_scalar(out=tmpy[:n, :], in0=vh, scalar1=SEQM[:n, b:b + 1], scalar2=None, op0=ALU.mult), r=['vv', 'CT'], w=['tmpy'])
                        for kt in range(2):
                            ps6, pt6 = nps()
                            P.pe(lambda e, ps6=ps6, kt=kt: e.matmul(ps6[:, :], k_e[:n, kt * 128:(kt + 1) * 128], tmpy[:n, :], start=True, stop=True), r=[('k_e', hb2), 'tmpy'], w=[pt6])
                            so = yoacc[:, kt * 512:(kt + 1) * 512]
                            P.dve(lambda e, ps6=ps6, kt=kt, b=b, so=so: e.scalar_tensor_tensor(out=so, in0=S0b[:, kt, :], scalar=decT[:, kt * 16 + b:kt * 16 + b + 1], in1=ps6[:, :], op0=ALU.mult, op1=ALU.add),
                                  r=['S0b', ('decT', hb2), pt6], w=['yoacc'])
                        P.dma('sp', gla_s[b, hd * 256:(hd + 1) * 256, :].rearrange("(k p) v -> p k v", p=128), yoacc[:, 0:1024].rearrange("p (k v) -> p k v", k=2), r=['yoacc'], w=[('gla_s',)], chan='x', is_out=True)
                    o_src, o_tok = tmp2[:n, :], 'tmp2'
                ybh = yb[:n, hd * 512:(hd + 1) * 512]
                P.act(lambda e, o_src=o_src, ybh=ybh, hd=hd: e.activation(out=ybh, in_=o_src, func=AF.Square, accum_out=stat[:n, 4 + hd:5 + hd]), r=[o_tok], w=['yb', 'stat'])
                rstd_from(stat[:n, 4 + hd:5 + hd], stat[:n, 8 + hd:9 + hd], n, 512.0, ['stat'])
                P.dve(lambda e, o_src=o_src, ybh=ybh, hd=hd: e.tensor_scalar(out=ybh, in0=o_src, scalar1=stat[:n, 8 + hd:9 + hd], scalar2=None, op0=ALU.mult),
                      r=[o_tok, 'stat'], w=['yb'])
                P.pool(lambda e, ybh=ybh, hd=hd: e.tensor_tensor(out=ybh, in0=ybh, in1=rr[:n, hd * 512:(hd + 1) * 512], op=ALU.mult), r=['yb', 'rr'], w=['yb'])
            if not smp:
                for kt in range(2):
                    ps7, pt7 = nps()
                    P.pe(lambda e, ps7=ps7, kt=kt, hd=hd: e.matmul(ps7[:, :], k_e[:n, kt * 128:(kt + 1) * 128], vv[:n, hd * 512:(hd + 1) * 512], start=True, stop=True), r=[('k_e', hb2), 'vv'], w=[pt7])
                    P.dve(lambda e, ps7=ps7, kt=kt, hd=hd: e.scalar_tensor_tensor(out=SG[:, hd * 2 + kt, :], in0=SG[:, hd * 2 + kt, :], scalar=decT[:, kt * 16:kt * 16 + 1], in1=ps7[:, :], op0=ALU.mult, op1=ALU.add),
                          r=[('SG', hd), ('decT', hb2), pt7], w=[('SG', hd)])
        for hd in range(4):
            head(hd)
        if mode != 'prefix':
            P.dma('sp', ymix[t0 - TP:t0 - TP + n, 2048:4096], yb[:n, :], r=['yb'], w=[('ymix',)], chan='x')

    for i in range(16):
        mode = 'prefix' if i < 8 else 'main'
        ssd_tile(i * 128, 128, mode)
        gla_chunk(i * 128, 64, mode)
        gla_chunk(i * 128 + 64, 64, mode)
        if i == 7:
            P.dve(lambda e: e.tensor_scalar(out=HT[:, :], in0=HT[:, :], scalar1=PM[:, 0:1], scalar2=None, op0=ALU.mult), r=[('HT', q) for q in range(8)] + ['PM'], w=[('HT', q) for q in range(8)])
            P.dve(lambda e: e.tensor_scalar(out=RB[:, 0:4096], in0=RB[:, 0:4096], scalar1=PM[:, 0:1], scalar2=None, op0=ALU.mult), r=[('SG', q) for q in range(4)] + ['PM'], w=[('SG', q) for q in range(4)])
    P.barrier()
    P.dma('sp', gla_p.rearrange("(a p) v -> p a v", p=128), SG, r=[('SG', q) for q in range(4)], w=[('gla_p',)], chan='x', is_out=True)
    for j0 in range(0, 16, 4):
        ps, pt = nps()
        for j in range(4):
            P.pe(lambda e, ps=ps, j=j, j0=j0: e.transpose(ps[:, j * 128:(j + 1) * 128], HT[:, (j0 + j) * 128:(j0 + j + 1) * 128], ident), r=[('HT', (j0 + j) // 2), 'CT'], w=[pt])
        P.dve(lambda e, ps=ps, j0=j0: e.tensor_copy(out=sstg[:, j0 * 128:(j0 + 4) * 128], in_=ps[:, :]), r=[pt], w=['sstg'])
    P.dma('sp', ssm_p.rearrange("(j q) n -> q j n", q=128), sstg.rearrange("p (j n) -> p j n", j=16), r=['sstg'], w=[('ssm_p',)], chan='x', is_out=True)
    P.barrier()
    ssd_tile(2048, 64, 'sample')
    P.barrier()
    gla_chunk(2048, 64, 'sample')
    g.ps_pool = list(range(8))
    if stop == 'mix':
        return finish()

    def rows_to_T(src, ncols, dstT, dst_tok, bufs, kofs=0, gains=None):
        for ti, (t0, n) in enumerate(MAINT):
            for h0c in range(0, ncols, 2048):
                b = bufs[(ti * (ncols // 2048) + h0c // 2048) % 2]
                btok = ('r2t', id(bufs), (ti * (ncols // 2048) + h0c // 2048) % 2)
                P.dma('sp', b[:n, :], src[t0:t0 + n, h0c:h0c + 2048], r=[('ymix',), ('xres',)], w=[btok], chan='x')
                if gains is not None:
                    gn, gnt = gains[h0c // 2048]
                    P.pool(lambda e, b=b, n=n, gn=gn: e.tensor_tensor(out=b[:n, :], in0=b[:n, :], in1=gn[:n, :], op=ALU.mult), r=[btok, gnt], w=[btok])
                transpose_to(lambda j0, nj, t0=t0, n=n, h0c=h0c: dstT[:, kofs + h0c // 128 + j0:kofs + h0c // 128 + j0 + nj, t0:t0 + n],
                             b, n, 2048, btok, dst_tok)

    def make_gate(l, j, Gp, Gs, tmp):
        gp_t = make_mod(Gp, l, j, 128, selP, tmp)
        gs_t = make_mod(Gs, l, j, 64, selS, tmp)
        return gp_t, gs_t

    def linear_res(hT, hT_tok, Wap, ksplit, ncols, Gp, Gs, gtoks, dg=None, NCB=256):
        if g.no_dg:
            dg = None
        for c0 in range(0, ncols, NCB):
            tl = []
            for (k0, kt) in ksplit:
                wi, wv, wt = W.get(Wap[k0 * 128:(k0 + kt) * 128, c0:c0 + NCB].rearrange("(k p) n -> p k n", p=128), kt, NCB)
                tl.append((wi, wv, wt, k0, kt))
            nk = sum(kt for (_, kt) in ksplit)
            for ti, (t0, n) in enumerate(MAINT):
                ps, pt = nps()
                kk = 0
                for (wi, wv, wt, k0, kt) in tl:
                    for k in range(kt):
                        P.pe(lambda e, ps=ps, k=k, k0=k0, t0=t0, n=n, wv=wv, kk=kk: e.matmul(
                            ps[:n, :NCB], hT[:, k0 + k, t0:t0 + n], wv[:, k, :], start=(kk == 0), stop=(kk == nk - 1)),
                            r=[hT_tok, wt], w=[pt])
                        kk += 1
                i = g.stg_i % 4
                g.stg_i += 1
                sg = STG[i]
                stok = ('stg', i)
                G = Gs if n == 64 else Gp
                gt = gtoks[1] if n == 64 else gtoks[0]
                if dg is None:
                    P.dve(lambda e, ps=ps, sg=sg, G=G, n=n, c0=c0: e.tensor_tensor(out=sg[:n, :NCB], in0=ps[:n, :NCB], in1=G[:n, c0:c0 + NCB], op=ALU.mult),
                          r=[pt, gt], w=[stok])
                else:
                    P.dve(lambda e, ps=ps, sg=sg, G=G, n=n, c0=c0, ti=ti: e.scalar_tensor_tensor(out=sg[:n, :NCB], in0=ps[:n, :NCB], scalar=dg(ti, n), in1=G[:n, c0:c0 + NCB],
                                                                                                op0=ALU.mult, op1=ALU.mult), r=[pt, gt, 'DG'], w=[stok])
                P.dma('pool', xres[t0:t0 + n, c0:c0 + NCB], sg[:n, :NCB], r=[stok, ('xres', ti, c0)], w=[('xres', ti, c0)], chan='acc', accum=True)
            for (wi, wv, wt, k0, kt) in tl:
                W.done(wi)

    def swiglu_T(hT, hT_tok, w1, w3, aT, s_tmp):
        TC = [(0, 512), (512, 512), (1024, 64)]
        for c0 in range(0, DFF, 256):
            ncb = min(256, DFF - c0)
            w1i, w1v, w1t = W.get(w1[:, c0:c0 + ncb].rearrange("(k p) n -> p k n", p=128), 16, ncb)
            w3i, w3v, w3t = W.get(w3[:, c0:c0 + ncb].rearrange("(k p) n -> p k n", p=128), 16, ncb)
            for jj in range(ncb // 128):
                f = c0 // 128 + jj
                for ci, (t0, tn) in enumerate(TC):
                    ps1, pt1 = nps()
                    for k in range(16):
                        P.pe(lambda e, ps1=ps1, k=k, t0=t0, tn=tn, jj=jj, w1v=w1v: e.matmul(ps1[:, :tn], w1v[:, k, jj * 128:(jj + 1) * 128], hT[:, k, t0:t0 + tn],
                                                                                           start=(k == 0), stop=(k == 15)), r=[hT_tok, w1t], w=[pt1])
                    ps3, pt3 = nps()
                    for k in range(16):
                        P.pe(lambda e, ps3=ps3, k=k, t0=t0, tn=tn, jj=jj, w3v=w3v: e.matmul(ps3[:, :tn], w3v[:, k, jj * 128:(jj + 1) * 128], hT[:, k, t0:t0 + tn],
                                                                                           start=(k == 0), stop=(k == 15)), r=[hT_tok, w3t], w=[pt3])
                    sb = s_tmp[(f * 3 + ci) % 2]
                    sbt = ('s_tmp', (f * 3 + ci) % 2)
                    P.act(lambda e, ps1=ps1, sb=sb, tn=tn: e.activation(out=sb[:, :tn], in_=ps1[:, :tn], func=AF.Silu), r=[pt1], w=[sbt])
                    P.dve(lambda e, ps3=ps3, sb=sb, tn=tn, f=f, t0=t0: e.tensor_tensor(out=aT[:, f, t0:t0 + tn], in0=sb[:, :tn], in1=ps3[:, :tn], op=ALU.mult),
                          r=[pt3, sbt], w=['aT'])
            W.done(w1i)
            W.done(w3i)

    P.barrier()
    yT = RB[:, 0:17408].bitcast(BF16).rearrange("p (k t) -> p k t", k=32)
    RBUF = [ra(0, 2048), ra(2048, 2048)]
    Gp, Gs, gtmp = ra(4096, 2048), ra(6144, 2048), ra(8192, 2048)
    ANORM, GNORM = ra(10240, 2048), ra(12288, 2048)
    load_bc_row(ANORM[:, :], R_AN, 'ANORM')
    load_bc_row(GNORM[:, :], R_GN, 'GNORM')
    rows_to_T(ymix, 4096, yT, 'yT', RBUF, gains=[(ANORM, 'ANORM'), (GNORM, 'GNORM')])
    gt = make_gate(0, 2, Gp, Gs, gtmp)
    linear_res(yT, 'yT', w_out0, [(0, 16), (16, 16)], D, Gp, Gs, gt)
    if stop == 'outproj':
        return finish()

    def ffn_block(l, j_shift, j_scale, j_gate, gamma_row, w1, w3, w2, router=None, dg=None, experts=None):
        P.barrier()
        hT = RB[:, 0:8704].bitcast(BF16).rearrange("p (k t) -> p k t", k=16)
        Gp2, Gs2 = rb(8704, 2048), rb(10752, 2048)
        s_tmp = [RB[:, 12800:13056].bitcast(BF16), RB[:, 13056:13312].bitcast(BF16)]
        S1 = dict(gam=ra(0, 2048), s1p=ra(2048, 2048), s2p=ra(4096, 2048), s1s=ra(6144, 2048), s2s=ra(8192, 2048),
                  tmp=ra(10240, 2048), stat=ra(12288, 8), h=[ra(12544, 2048), ra(14592, 2048)],
                  hTf=[ra(20736, 512), ra(21248, 512)])
        XB3 = [ra(16640, 2048), ra(18688, 2048)]
        norm_to_hT(lambda t0, n: xres[t0:t0 + n, :], MAINT, hT, 'hT', l, j_shift, j_scale, gamma_row, S1, XB3, router=router)
        gt2 = make_gate(l, j_gate, Gp2, Gs2, S1['tmp'])
        if router is not None:
            dump('d_DG', DG_ap[0][:, :, :], [128, 9, 8], F32, r=['DG'])
        if experts is not None and stop == 'router':
            return 'stop'
        P.barrier()
        aT = RA[:, 0:23392].bitcast(BF16).rearrange("p (f t) -> p f t", f=43)
        if experts is None:
            swiglu_T(hT, 'hT', w1, w3, aT, s_tmp)
            linear_res(aT, 'aT', w2, [(0, 15), (15, 14), (29, 14)], D, Gp2, Gs2, gt2)
        else:
            for ex in range(g.nexp_dbg):
                swiglu_T(hT, 'hT', w1[ex], w3[ex], aT, s_tmp)
                linear_res(aT, 'aT', w2[ex], [(0, 15), (15, 14), (29, 14)], D, Gp2, Gs2, gt2, dg=(lambda ti, n, ex=ex: dg(ti, n, ex)))

    ffn_block(0, 3, 4, 5, R_NF0, ffn_w1, ffn_w3, ffn_w2)
    if stop == 'ffn0':
        return finish()

    P.barrier()
    hT1 = RB[:, 0:8704].bitcast(BF16).rearrange("p (k t) -> p k t", k=16)
    S2 = dict(gam=ra(0, 2048), s1p=ra(2048, 2048), s2p=ra(4096, 2048), s1s=ra(6144, 2048), s2s=ra(8192, 2048),
              tmp=ra(10240, 2048), stat=ra(12288, 8), h=[ra(12544, 2048), ra(14592, 2048)])
    XB4 = [ra(16640, 2048), ra(18688, 2048)]
    norm_to_hT(lambda t0, n: xres[t0:t0 + n, :], MAINT, hT1, 'hT', 1, 0, 1, R_NM1, S2, XB4)
    P.barrier()

    def evac_cu(ps, pt, ti, t0, n, c0, ncb):
        i = g.stg_i % 4
        g.stg_i += 1
        sg = STG[i]
        stok = ('stg', i)
        P.act(lambda e: e.activation(out=sg[:n, :ncb], in_=ps[:n, :ncb], func=AF.Gelu), r=[pt], w=[stok])
        P.dma('sp', cu[t0:t0 + n, c0:c0 + ncb], sg[:n, :ncb], r=[stok], w=[('cu',)], chan='x')
    linear_A(hT1, 'hT', MAINT, c_w_in, D, [(c, 256) for c in range(0, 4 * D, 256)], evac_cu)
    P.barrier()
    ub, vb, vn, yc = ra(0, 4096), ra(4096, 4096), ra(8192, 4096), ra(12288, 4096)
    LG, LB = ra(16384, 4096), rb(8704, 4096)
    WST = rb(12800, 1024).rearrange("p (g t) -> p g t", g=8)
    WSTs = rb(13824, 512).rearrange("p (g t) -> p g t", g=8)
    CBS, CBSs = rb(14336, 8), rb(14344, 8)
    st1 = rb(14352, 32)
    P.dma('sp', LG[:, 0:2048], rows_d[R_LG0:R_LG0 + 1, :].partition_broadcast(128), w=['LG'], chan='x')
    P.dma('sp', LG[:, 2048:4096], rows_d[R_LG1:R_LG1 + 1, :].partition_broadcast(128), w=['LG'], chan='x')
    P.dma('sp', LB[:, 0:2048], rows_d[R_LB0:R_LB0 + 1, :].partition_broadcast(128), w=['LB'], chan='x')
    P.dma('sp', LB[:, 2048:4096], rows_d[R_LB1:R_LB1 + 1, :].partition_broadcast(128), w=['LB'], chan='x')
    P.dma('sp', WST, wsT_d, w=['WST'], chan='x')
    P.dma('sp', WSTs[:64], wsTs_d, w=['WSTs'], chan='x')
    P.dma('sp', CBS[:, :], cbs_d, w=['CBS'], chan='x')
    P.dma('sp', CBSs[:64, :], cbss_d, w=['CBS'], chan='x')
    P.dve(lambda e: e.tensor_tensor(out=WST, in0=WST, in1=CT[:, K_CM:K_CM + 128].unsqueeze(1).to_broadcast([128, 8, 128]), op=ALU.mult), r=['WST', 'CT'], w=['WST'])
    P.dve(lambda e: e.tensor_tensor(out=WSTs[:64], in0=WSTs[:64], in1=CT[:64, K_CMS:K_CMS + 64].unsqueeze(1).to_broadcast([64, 8, 64]), op=ALU.mult), r=['WSTs', 'CT'], w=['WSTs'])
    def cmix_tile(ti, t0, n):
        smp = (n == 64)
        P.dma('sp', ub[:n, :], cu[t0:t0 + n, 0:4096], r=[('cu',)], w=['ub'], chan='x')
        P.dma('sp', vb[:n, :], cu[t0:t0 + n, 4096:8192], r=[('cu',)], w=['vb'], chan='x')
        for gq in range(8):
            vg = vb[:n, gq * 512:(gq + 1) * 512]
            ng = vn[:n, gq * 512:(gq + 1) * 512]
            P.act(lambda e, vg=vg, ng=ng, gq=gq: e.activation(out=ng, in_=vg, func=AF.Copy, accum_out=st1[:n, gq:gq + 1]), r=['vb'], w=['vn', 'st1'])
            P.dve(lambda e, gq=gq: e.tensor_scalar(out=st1[:n, 8 + gq:9 + gq], in0=st1[:n, gq:gq + 1], scalar1=-1.0 / 512.0, scalar2=None, op0=ALU.mult), r=['st1'], w=['st1'])
            P.act(lambda e, vg=vg, gq=gq: e.activation(out=vg, in_=vg, func=AF.Identity, bias=st1[:n, 8 + gq:9 + gq], scale=1.0), r=['vb', 'st1'], w=['vb'])
            P.act(lambda e, vg=vg, ng=ng, gq=gq: e.activation(out=ng, in_=vg, func=AF.Square, accum_out=st1[:n, 16 + gq:17 + gq]), r=['vb'], w=['vn', 'st1'])
            P.act(lambda e, gq=gq: e.activation(out=st1[:n, 24 + gq:25 + gq], in_=st1[:n, 16 + gq:17 + gq], func=AF.Sqrt, scale=1.0 / 512.0, bias=CT[:n, K_EPS:K_EPS + 1]), r=['st1', 'CT'], w=['st1'])
            P.dve(lambda e, gq=gq: e.reciprocal(out=st1[:n, 24 + gq:25 + gq], in_=st1[:n, 24 + gq:25 + gq]), r=['st1'], w=['st1'])
            P.dve(lambda e, vg=vg, ng=ng, gq=gq: e.scalar_tensor_tensor(out=ng, in0=vg, scalar=st1[:n, 24 + gq:25 + gq], in1=LG[:n, gq * 512:(gq + 1) * 512], op0=ALU.mult, op1=ALU.mult),
                  r=['vb', 'st1', 'LG'], w=['vn'])
            P.pool(lambda e, ng=ng, gq=gq: e.tensor_tensor(out=ng, in0=ng, in1=LB[:n, gq * 512:(gq + 1) * 512], op=ALU.add), r=['vn', 'LB'], w=['vn'])
        if smp:
            P.dma('sp', cmlp_v, vn[:n, :], r=['vn'], w=[('cmlpv',)], chan='x', is_out=True)
        for gq in range(8):
            ps, pt = nps()
            lhs = WSTs[:64, gq, :] if smp else WST[:, gq, :]
            P.pe(lambda e, ps=ps, lhs=lhs, gq=gq: e.matmul(ps[:n, :], lhs, vn[:n, gq * 512:(gq + 1) * 512], start=True, stop=True), r=['WST', 'WSTs', 'vn'], w=[pt])
            bsc = (CBSs if smp else CBS)[:n, gq:gq + 1]
            P.dve(lambda e, ps=ps, gq=gq, bsc=bsc: e.scalar_tensor_tensor(out=yc[:n, gq * 512:(gq + 1) * 512], in0=ps[:n, :], scalar=bsc, in1=ub[:n, gq * 512:(gq + 1) * 512], op0=ALU.add, op1=ALU.mult),
                  r=[pt, 'CBS', 'ub'], w=['yc'])
        P.dma('sp', ymix[t0:t0 + n, :], yc[:n, :], r=['yc'], w=[('ymix',)], chan='x')
    for ti, (t0, n) in enumerate(MAINT):
        cmix_tile(ti, t0, n)
    P.barrier()
    yT1 = RB[:, 0:17408].bitcast(BF16).rearrange("p (k t) -> p k t", k=32)
    rows_to_T(ymix, 4096, yT1, 'yT', RBUF)
    gt1 = make_gate(1, 2, Gp, Gs, gtmp)
    linear_res(yT1, 'yT', c_w_out, [(0, 16), (16, 16)], D, Gp, Gs, gt1)
    if stop == 'mixc':
        return finish()

    if 'noalloc' in g.rflags:
        DG = rb(13312, 72).rearrange("p (a b) -> p a b", a=9)
        WR = rb(13440, 128).rearrange("p (a b) -> p a b", a=16)
        RS = rb(13568, 64)
    else:
        DG = st.enter_context(nc.sbuf_tensor("DG", [128, 9, 8], F32))
        WR = st.enter_context(nc.sbuf_tensor("WR", [128, 16, 8], F32))
        RS = st.enter_context(nc.sbuf_tensor("RS", [128, 64], F32))
    if 'nowr' not in g.rflags:
        P.dma('sp', WR[:, :, :], router_d.rearrange("(k p) e -> p k e", p=128), w=['WR'], chan='x')
    hTm = RB[:, 0:8704].bitcast(BF16).rearrange("p (k t) -> p k t", k=16)
    HTF = [ra(20736, 512), ra(21248, 512)]

    def router(ti, t0, n, hb, htok):
        psr, ptr = PS[7], ('ps', 7)
        for j0 in range(0, 16, 4):
            ps, pt = nps()
            for j in range(4):
                P.pe(lambda e, ps=ps, j=j, j0=j0: e.transpose(ps[:, j * 128:j * 128 + n], hb[:n, (j0 + j) * 128:(j0 + j + 1) * 128], ident[:n, :n]), r=[htok, 'CT'], w=[pt])
            hf = HTF[(j0 // 4) % 2]
            hft = ('HTF', (j0 // 4) % 2)
            src = ps[:, 0:512].rearrange("p (a b) -> p a b", a=4)[:, :, 0:n]
            hf3 = hf.rearrange("p (a b) -> p a b", a=4)[:, :, 0:n]
            P.act(lambda e, src=src, hf3=hf3: e.activation(out=hf3, in_=src, func=AF.Copy), r=[pt], w=[hft])
            P.dve(lambda e, hf3=hf3, j0=j0: e.tensor_copy(out=hTm[:, j0:j0 + 4, t0:t0 + n], in_=hf3), r=[hft], w=['hT'])
            for j in range(4 if g.rmode < 2 else 0):
                P.pe(lambda e, j=j, j0=j0, hf=hf: e.matmul(psr[:n, 0:8], hf[:, j * 128:j * 128 + n], WR[:, j0 + j, :], start=(j0 + j == 0), stop=(j0 + j == 15)), r=[hft, 'WR'], w=[ptr])
        if g.rmode >= 1:
            if 'nodg' not in g.rflags:
                P.dve(lambda e, ti=ti: e.memset(DG[:n, ti, :], 0.125), w=['DG'])
            return
        lg, mx, ngv, ex, msk, den = RS[:n, 0:8], RS[:n, 8:16], RS[:n, 16:17], RS[:n, 24:32], RS[:n, 32:40], RS[:n, 40:41]
        P.dve(lambda e: e.tensor_copy(out=lg, in_=psr[:n, 0:8]), r=[ptr], w=['RS'])
        P.dve(lambda e: e.tensor_reduce(out=mx[:, 0:1], in_=lg, axis=mybir.AxisListType.X, op=ALU.max), r=['RS'], w=['RS'])
        P.dve(lambda e: e.tensor_scalar(out=msk, in0=lg, scalar1=mx[:, 0:1], scalar2=-1e30, op0=ALU.is_equal, op1=ALU.mult), r=['RS'], w=['RS'])
        P.dve(lambda e: e.tensor_tensor(out=msk, in0=msk, in1=lg, op=ALU.add), r=['RS'], w=['RS'])
        P.dve(lambda e: e.tensor_reduce(out=mx[:, 1:2], in_=msk, axis=mybir.AxisListType.X, op=ALU.max), r=['RS'], w=['RS'])
        P.dve(lambda e: e.tensor_scalar(out=ngv, in0=mx[:, 0:1], scalar1=-1.0, scalar2=None, op0=ALU.mult), r=['RS'], w=['RS'])
        P.act(lambda e: e.activation(out=ex, in_=lg, func=AF.Exp, bias=ngv, scale=1.0), r=['RS'], w=['RS'])
        P.dve(lambda e: e.tensor_scalar(out=msk, in0=lg, scalar1=mx[:, 1:2], scalar2=None, op0=ALU.is_ge), r=['RS'], w=['RS'])
        P.dve(lambda e: e.tensor_tensor(out=ex, in0=ex, in1=msk, op=ALU.mult), r=['RS'], w=['RS'])
        P.dve(lambda e: e.tensor_reduce(out=den, in_=ex, axis=mybir.AxisListType.X, op=ALU.add), r=['RS'], w=['RS'])
        P.dve(lambda e: e.reciprocal(out=den, in_=den), r=['RS'], w=['RS'])
        P.dve(lambda e, ti=ti: e.tensor_scalar(out=DG[:n, ti, :], in0=ex, scalar1=den, scalar2=None, op0=ALU.mult), r=['RS'], w=['DG'])

    g.ps_pool = list(range(7))
    DG_ap.append(DG)
    rr_ = ffn_block(1, 3, 4, 5, R_NF1, moe_w1, moe_w3, moe_w2, router=(None if ('nocb' in g.rflags and stop == 'router') else router), dg=(lambda ti, n, ex: DG[:n, ti, ex:ex + 1]), experts=True)
    g.ps_pool = list(range(8))
    if stop in ('moe', 'router'):
        return finish()
    P.barrier()
    norm_to_hT(lambda t0, n: xres[t0:t0 + n, :], MAINT, None, None, 1, 0, 0, R_NFIN, S2, XB4, with_mod=False,
               out_rows=lambda t0, n: y_out[t0:t0 + n, :])
    return finish()

def make_consts():
    c = np.zeros((128, NCONST), np.float32)
    i = np.arange(128)
    c[:, K_ID:K_ID + 128] = np.eye(128)
    u = i[:, None]; t = i[None, :]
    c[:, K_L:K_L + 128] = (u > t)
    c[:, K_R:K_R + 128] = (u <= t)
    c[:, K_CM:K_CM + 128] = (u <= t)
    c[:, K_BLK:K_BLK + 128] = 1.0
    same = (u // 4 == t // 4)
    c[:, K_LS:K_LS + 128] = (u > t) & same
    c[:, K_RS:K_RS + 128] = (u <= t) & same
    c[:, K_CMS:K_CMS + 128] = (u <= t) & same
    c[:, K_BLKS:K_BLKS + 128] = same
    c[:, K_SEQM:K_SEQM + 16] = (i[:, None] // 4 == np.arange(16)[None, :])
    c[:, K_PICK:K_PICK + 16] = (i[:, None] == 4 * np.arange(16)[None, :])
    c[:, K_ONE:K_ONE + 128] = 1.0
    c[0, K_SELP:K_SELP + 128] = 1.0
    c[1:, K_SELP:K_SELP + 128] = 0.0
    for b in range(16):
        c[1 + b, K_SELS + 4 * b:K_SELS + 4 * b + 4] = 1.0
    c[:, K_EPS] = EPS
    return c


def prep_core(inp, c):
    f = np.float32
    seq, half = c // 2, c % 2
    xp = inp['x_prompt'][seq]
    main = xp[half * TP:(half + 1) * TP]
    prefix = xp[0:TP]
    xs = inp['x_sample'][16 * c:16 * c + 16].reshape(TS, D)
    xall = np.concatenate([prefix, main, xs], 0)
    call = np.concatenate([inp['c_prompt'][seq:seq + 1], inp['c_sample'][16 * c:16 * c + 16]], 0)
    rows = np.zeros((NROW, D), f)
    rows[R_NM0] = inp['norm_mix0'][0]; rows[R_NF0] = inp['norm_ffn0'][0]
    rows[R_AN] = inp['a_norm'][0]; rows[R_GN] = inp['gla_norm'][0]
    rows[R_NM1] = inp['norm_mix1'][0]; rows[R_NF1] = inp['norm_ffn1'][0]; rows[R_NFIN] = inp['norm_f']
    rows[R_LG0] = inp['c_ln_g'][0][:D]; rows[R_LG1] = inp['c_ln_g'][0][D:]
    rows[R_LB0] = inp['c_ln_b'][0][:D]; rows[R_LB1] = inp['c_ln_b'][0][D:]
    rows[R_BA, :1024] = inp['gla_ba'][0]
    cw = inp['conv_w'][0]; cb = inp['conv_b'][0]
    convp = np.zeros((128, 32, 5), f)
    convp[:, :, :4] = cw.reshape(4, 32, 128).transpose(2, 1, 0)
    convp[:, :, 4] = cb.reshape(32, 128).T
    hp = np.zeros((128, 3, 32), f)
    hp[:, 0] = inp['dt_bias'][0]; hp[:, 1] = inp['a_log'][0]; hp[:, 2] = inp['d_skip'][0]
    ws = inp['c_ws'][0]; bs = inp['c_bs'][0]
    wsT = np.ascontiguousarray(ws.transpose(2, 0, 1))
    wsTs = np.zeros((64, 8, 64), f)
    cbss = np.zeros((64, 8), f)
    for b in range(16):
        wsTs[4 * b:4 * b + 4, :, 4 * b:4 * b + 4] = wsT[:4, :, :4]
        cbss[4 * b:4 * b + 4] = bs[:, :4].T
    m = dict(
        xall=xall, call=call, pmask=np.full((128, 1), float(half), f), consts=make_consts(), rows=rows,
        convp=convp, hp=hp, gla_wa2=inp['gla_wa2'][0],
        sssm=inp['state_ssm'][0, 16 * c:16 * c + 16].reshape(16, 2048, 128),
        sconv=inp['state_conv'][0, 16 * c:16 * c + 16],
        sgla=inp['state_gla'][0, 16 * c:16 * c + 16].reshape(16, 1024, 512),
        ada_w0=inp['ada_w0'][0], ada_w1=inp['ada_w1'][0], ada_b0=inp['ada_b0'], ada_b1=inp['ada_b1'],
        w_in0=inp['w_in0'][0], w_out0=inp['w_out0'][0], ffn_w1=inp['ffn_w1'][0], ffn_w3=inp['ffn_w3'][0],
        ffn_w2=inp['ffn_w2'][0], c_w_in=inp['c_w_in'][0], c_w_out=inp['c_w_out'][0],
        wsT=wsT, wsTs=wsTs, cbs=np.ascontiguousarray(bs.T), cbss=cbss, router_w=inp['router_w'][0],
    )
    for i in range(NEXP):
        m['moe_w1_%d' % i] = inp['moe_w1'][0][i]
        m['moe_w3_%d' % i] = inp['moe_w3'][0][i]
        m['moe_w2_%d' % i] = inp['moe_w2'][0][i]
    return {k: np.ascontiguousarray(np.asarray(v, dtype=np.float32)) for k, v in m.items()}


def kernel(**inputs):
    inp = {k: np.asarray(v) for k, v in inputs.items()}
    nc = build()
    in_maps = [prep_core(inp, c) for c in range(8)]
    res = run_bass_kernel_spmd(nc, in_maps, core_ids=list(range(8)))
    outs = res.results
    f = np.float32
    y_prompt = np.zeros((4, 2048, D), f)
    y_sample = np.zeros((128, 4, D), f)
    ssm_prompt = np.zeros((1, 4, 32, 64, 128), f)
    conv_prompt = np.zeros((1, 4, 3, 4096), f)
    gla_prompt = np.zeros((1, 4, 4, 256, 512), f)
    ssm_sample = np.zeros((1, 128, 32, 64, 128), f)
    conv_sample = np.zeros((1, 128, 3, 4096), f)
    gla_sample = np.zeros((1, 128, 4, 256, 512), f)
    cmlp = np.zeros((1, 128, 4, 8, 512), f)
    for c in range(8):
        o = outs[c]
        seq, half = c // 2, c % 2
        y_prompt[seq, half * TP:(half + 1) * TP] = o['y_out'][:TP]
        y_sample[16 * c:16 * c + 16] = o['y_out'][TP:].reshape(16, 4, D)
        if half == 1:
            ssm_prompt[0, seq] = o['ssm_p'].reshape(32, 64, 128)
            conv_prompt[0, seq] = o['conv_p']
            gla_prompt[0, seq] = o['gla_p'].reshape(4, 256, 512)
        ssm_sample[0, 16 * c:16 * c + 16] = o['ssm_s'].reshape(16, 32, 64, 128)
        conv_sample[0, 16 * c:16 * c + 16] = o['conv_s']
        gla_sample[0, 16 * c:16 * c + 16] = o['gla_s'].reshape(16, 4, 256, 512)
        cmlp[0, 16 * c:16 * c + 16] = o['cmlp_v'].reshape(16, 4, 8, 512)
    return (y_prompt, y_sample, ssm_prompt, conv_prompt, gla_prompt, ssm_sample, conv_sample, gla_sample, cmlp)
```

```python
import contextlib
import numpy as np
import concourse.bass as bass
import concourse.mybir as mybir
from concourse.bass_utils import run_bass_kernel_spmd

F32 = mybir.dt.float32
BF16 = mybir.dt.bfloat16
AF = mybir.ActivationFunctionType
ALU = mybir.AluOpType

D = 2048
TP = 1024
TS = 64
TA = 2112
TM = 1088
NSEQ = 17
DFF = 5504
NIN = 12336
NEXP = 8
EPS = 1e-6
NSLOT = 4
SLOT_ELEMS = 4096

C_Z, C_XBC, C_DT, C_Q, C_K, C_V, C_R, C_G = 0, 2048, 6144, 6176, 7200, 8224, 10272, 12320

K_ID, K_L, K_R, K_CM, K_BLK, K_LS, K_RS, K_CMS, K_BLKS, K_SEQM, K_PICK, K_ONE = (
    0, 128, 256, 384, 512, 640, 768, 896, 1024, 1152, 1168, 1184)
K_SELP, K_SELS, K_EPS = 1312, 1440, 1504
NCONST = 1512
R_NM0, R_NF0, R_AN, R_GN, R_NM1, R_NF1, R_NFIN, R_LG0, R_LG1, R_LB0, R_LB1, R_BA = range(12)
NROW = 12


class Prog:
    def __init__(self, nc):
        self.nc = nc
        self.ops = []
        self.out_chans = set()
        self.all_chans = set()

    def add(self, eng, fn, r=(), w=(), chan=None):
        self.ops.append(dict(eng=eng, fn=fn, r=tuple(r), w=tuple(w), chan=chan))
        return len(self.ops) - 1

    def pe(self, fn, r=(), w=()):
        return self.add('pe', fn, r, w)

    def dve(self, fn, r=(), w=()):
        return self.add('dve', fn, r, w)

    def act(self, fn, r=(), w=()):
        return self.add('act', fn, r, w)

    def pool(self, fn, r=(), w=()):
        return self.add('pool', fn, r, w)

    def barrier(self):
        self.ops.append(dict(eng='bar', fn=None, r=(), w=(), chan=None))

    def dma(self, eng, out, in_, r=(), w=(), chan=None, is_out=False, mode_all=False, accum=False):
        if is_out:
            self.out_chans.add(chan)
        if mode_all:
            self.all_chans.add(chan)
        if accum:
            fn = lambda e, o=out, i=in_: e.dma_start(out=o, in_=i, accum_op=ALU.add)
        else:
            fn = lambda e, o=out, i=in_: e.dma_start(out=o, in_=i)
        return self.add(eng, fn, r, w, chan=chan)

    def emit(self):
        nc = self.nc
        ops = self.ops
        last_w = {}
        readers = {}
        last_eng = {}
        last_chan = {}
        pend = {}
        RING = {'sp': 16, 'pool': 8, 'act': 4}
        q_n0 = {}
        last_ring = {}
        for op in ops:
            if op['eng'] != 'bar' and op['chan'] is not None:
                m = q_n0.get(op['eng'], 0)
                q_n0[op['eng']] = m + 1
                op['ring0'] = (op['eng'], m % RING[op['eng']])
        for i, op in enumerate(ops):
            raw = set()
            deps = set()
            if op['eng'] == 'bar':
                bd = set(last_eng.values()) | set(last_chan.values()) | set(last_ring.values())
                for en in ['pe', 'dve', 'act', 'pool', 'sp']:
                    pend[en] = set(pend.get(en, set())) | bd
                op['deps'] = set()
                continue
            if pend.get(op['eng']):
                deps |= pend[op['eng']]
                raw |= pend[op['eng']]
                pend[op['eng']] = set()
            if op['chan'] is not None:
                last_chan[op['chan']] = i
                last_ring[op['ring0']] = i
            else:
                last_eng[op['eng']] = i
            for t in op['r']:
                raw |= set(last_w.get(t, {}).values())
            for t in op['w']:
                deps |= set(last_w.get(t, {}).values())
                deps |= set(readers.get(t, {}).values())
            deps |= raw
            deps.discard(i)
            keep = set()
            for j in deps:
                pj = ops[j]
                if pj['chan'] is None and pj['eng'] == op['eng']:
                    if op['eng'] == 'pe':
                        continue
                    if j not in raw:
                        continue
                keep.add(j)
            op['deps'] = keep
            key = ('c', op['chan']) if op['chan'] is not None else ('e', op['eng'])
            for t in op['r']:
                readers.setdefault(t, {})[key] = i
            for t in op['w']:
                last_w.setdefault(t, {})[key] = i
        signal = set()
        for op in ops:
            for j in op['deps']:
                if ops[j]['chan'] is None:
                    signal.add(j)
        eng_cnt = {}
        chan_cnt = {}
        q_n = {}
        for i, op in enumerate(ops):
            if op['eng'] == 'bar':
                op['ev'] = None
                continue
            if op['chan'] is not None:
                q = op['eng']
                m = q_n.get(q, 0)
                q_n[q] = m + 1
                c = (q, m % RING[q])
                assert c == op['ring0']
                chan_cnt[c] = chan_cnt.get(c, 0) + 1
                op['ev'] = ('c', c, 16 * chan_cnt[c])
                op['ring_prev'] = 16 * (chan_cnt[c] - 1)
                op['ring'] = c
            elif i in signal:
                e = op['eng']
                eng_cnt[e] = eng_cnt.get(e, 0) + 1
                op['ev'] = ('e', e, eng_cnt[e])
            else:
                op['ev'] = None
        engs = ['pe', 'dve', 'act', 'pool', 'sp']
        stack = contextlib.ExitStack()
        sems = {}
        for e in engs:
            if e in eng_cnt:
                sems[('e', e)] = stack.enter_context(nc.semaphore('s_' + e))
        for c in chan_cnt:
            sems[('c', c)] = stack.enter_context(nc.semaphore('c_%d' % len(sems)))
        self.n_sems = len(sems)
        self.stats = (dict(eng_cnt), dict(chan_cnt), len(ops))
        by_eng = {e: [] for e in engs}
        for i, op in enumerate(ops):
            if op['eng'] != 'bar':
                by_eng[op['eng']].append(i)
        out_final = [(sems[('c', c)], 16 * chan_cnt[c]) for c in chan_cnt]

        def run_engine(ename, e):
            seen = {}
            for i in by_eng[ename]:
                op = ops[i]
                need = {}
                for j in op['deps']:
                    ev = ops[j]['ev']
                    k = (ev[0], ev[1])
                    need[k] = max(need.get(k, 0), ev[2])
                for k, v in need.items():
                    if seen.get(k, 0) >= v:
                        continue
                    e.wait_ge(sems[k], v)
                    seen[k] = v
                if op['chan'] is not None and op['ring_prev'] > 0:
                    k = ('c', op['ring'])
                    if seen.get(k, 0) < op['ring_prev']:
                        e.wait_ge(sems[k], op['ring_prev'])
                        seen[k] = op['ring_prev']
                ins = op['fn'](e)
                ev = op['ev']
                if op['chan'] is not None:
                    ins.then_inc(sems[('c', op['ring'])], 16)
                elif ev is not None:
                    ins.then_inc(sems[('e', ename)], 1)
            if ename == 'sp':
                for s, v in out_final:
                    e.wait_ge(s, v)

        with stack:
            with nc.Block() as block:
                @block.tensor
                def _(e):
                    run_engine('pe', e)

                @block.vector
                def _(e):
                    run_engine('dve', e)

                @block.scalar
                def _(e):
                    run_engine('act', e)

                @block.gpsimd
                def _(e):
                    run_engine('pool', e)

                @block.sync
                def _(e):
                    run_engine('sp', e)
        return nc


class WStream:
    def __init__(self, P, slots):
        self.P = P
        self.slots = slots
        self.tiles = []

    def get(self, src_ap, kt, ncols):
        i = len(self.tiles)
        s = i % NSLOT
        view = self.slots[s][:, 0:kt * ncols].rearrange("p (k n) -> p k n", k=kt)
        self.tiles.append(dict(src=src_ap, view=view, start=len(self.P.ops), end=None, slot=s))
        return i, view, ('wslot', s)

    def done(self, i):
        self.tiles[i]['end'] = len(self.P.ops)

    def finalize(self):
        P = self.P
        ins = []
        for i, t in enumerate(self.tiles):
            pos = 0 if i < NSLOT else self.tiles[i - NSLOT]['end']
            op = dict(eng='pool',
                      fn=(lambda e, o=t['view'], s=t['src']: e.dma_start(out=o, in_=s)),
                      r=(), w=(('wslot', t['slot']),), chan=('w', t['slot']))
            ins.append((pos, i, op))
        ins.sort(key=lambda x: (x[0], x[1]))
        new = []
        k = 0
        for idx in range(len(P.ops) + 1):
            while k < len(ins) and ins[k][0] == idx:
                new.append(ins[k][2])
                k += 1
            if idx < len(P.ops):
                new.append(P.ops[idx])
        P.ops = new


class B:
    pass


def build(debug=(), stop=None, dumps=None, nexp_dbg=NEXP, no_dg=False, rmode=0, rflags=(), lite=False):
    nc = bass.Bass("TRN2", target_bir_lowering=False)
    P = Prog(nc)
    st = contextlib.ExitStack()
    g = B()
    g.nexp_dbg = nexp_dbg
    g.no_dg = no_dg
    g.rmode = rmode
    g.rflags = set(rflags)
    DG_ap = []

    def din(name, shape):
        return nc.dram_tensor(name, list(shape), F32, kind="ExternalInput").ap()

    def dout(name, shape):
        return nc.dram_tensor(name, list(shape), F32, kind="ExternalOutput").ap()

    def dscr(name, shape):
        kind = "ExternalOutput" if name in debug else "Internal"
        return nc.dram_tensor(name, list(shape), F32, kind=kind).ap()

    xall = din("xall", [TA, D])
    call = din("call", [NSEQ, D])
    pmask_d = din("pmask", [128, 1])
    consts_d = din("consts", [128, NCONST])
    rows_d = din("rows", [NROW, D])
    convp_d = din("convp", [128, 32, 5])
    hp_d = din("hp", [128, 3, 32])
    wa2_d = din("gla_wa2", [16, 1024])
    sssm = din("sssm", [16, 2048, 128])
    sconv = din("sconv", [16, 3, 4096])
    sgla = din("sgla", [16, 1024, 512])
    ada_w = [din("ada_w0", [D, 6 * D]), din("ada_w1", [D, 6 * D])]
    ada_b = [din("ada_b0", [1, 6 * D]), din("ada_b1", [1, 6 * D])]
    w_in0 = din("w_in0", [D, NIN])
    w_out0 = din("w_out0", [2 * D, D])
    ffn_w1 = din("ffn_w1", [D, DFF])
    ffn_w3 = din("ffn_w3", [D, DFF])
    ffn_w2 = din("ffn_w2", [DFF, D])
    c_w_in = din("c_w_in", [D, 4 * D])
    c_w_out = din("c_w_out", [2 * D, D])
    wsT_d = din("wsT", [128, 8, 128])
    wsTs_d = din("wsTs", [64, 8, 64])
    cbs_d = din("cbs", [128, 8])
    cbss_d = din("cbss", [64, 8])
    router_d = din("router_w", [D, NEXP])
    nw = 0 if lite else NEXP
    moe_w1 = [din("moe_w1_%d" % i, [D, DFF]) for i in range(nw)]
    moe_w3 = [din("moe_w3_%d" % i, [D, DFF]) for i in range(nw)]
    moe_w2 = [din("moe_w2_%d" % i, [DFF, D]) for i in range(nw)]

    y_out = dout("y_out", [TM, D])
    ssm_p = dout("ssm_p", [2048, 128])
    conv_p = dout("conv_p", [3, 4096])
    gla_p = dout("gla_p", [1024, 512])
    ssm_s = dout("ssm_s", [16, 2048, 128])
    conv_s = dout("conv_s", [16, 3, 4096])
    gla_s = dout("gla_s", [16, 1024, 512])
    cmlp_v = dout("cmlp_v", [TS, 4096])

    mod_d = [dscr("mod0", [NSEQ, 6 * D]), dscr("mod1", [NSEQ, 6 * D])]
    xres = dscr("xres", [TM, D])
    proj = dscr("proj", [TA, NIN])
    xbcT = dscr("xbcT", [4096, TA])
    ymix = dscr("ymix", [TM, 2 * D])
    cu = dscr("cu", [TM, 4 * D])
    moe_acc = dscr("moe_acc", [TM, D])

    RA = st.enter_context(nc.sbuf_tensor("RA", [128, 23936], F32))
    RB = st.enter_context(nc.sbuf_tensor("RB", [128, 17408], F32))
    WSL = [st.enter_context(nc.sbuf_tensor("ws%d" % i, [128, SLOT_ELEMS], BF16)) for i in range(NSLOT)]
    CT = st.enter_context(nc.sbuf_tensor("CT", [128, NCONST], F32))
    PM = st.enter_context(nc.sbuf_tensor("PM", [128, 1], F32))
    HP = st.enter_context(nc.sbuf_tensor("HP", [128, 3, 32], F32))
    SMALL = st.enter_context(nc.sbuf_tensor("SMALL", [128, 1024], F32))
    PS = [st.enter_context(nc.psum_tensor("ps%d" % i, [128, 512], F32)) for i in range(8)]
    g.ps_i = 0
    g.uid = 0

    g.ps_pool = list(range(8))

    def nps():
        pool = g.ps_pool
        i = pool[g.ps_i % len(pool)]
        g.ps_i += 1
        return PS[i], ('ps', i)

    def uid(p='t'):
        g.uid += 1
        return (p, g.uid)

    def ra(off, n):
        return RA[:, off:off + n]

    def rb(off, n):
        return RB[:, off:off + n]

    W = WStream(P, WSL)

    def finish():
        W.finalize()
        with st:
            P.emit()
        nc._prog_stats = P.stats
        return nc

    def dump(name, ap, shape, dt=F32, r=()):
        if dumps is None or name not in dumps:
            return
        d = nc.dram_tensor(name, list(shape), dt, kind="ExternalOutput").ap()
        P.dma('sp', d, ap, r=list(r), chan=('dump', name), is_out=True)

    ident = CT[:, K_ID:K_ID + 128]
    ones_row = CT[0:1, K_ONE:K_ONE + 128]

    P.dma('sp', CT[:], consts_d, w=['CT'], chan='c0', mode_all=True)
    P.dma('sp', PM[:], pmask_d, w=['PM'], chan='c0', mode_all=True)
    P.dma('sp', HP[:], hp_d, w=['HP'], chan='c0', mode_all=True)

    def transpose_to(dst_fn, src, n, ncol, src_tok, dst_tok, eng_alt=[0]):
        nb = ncol // 128
        for j0 in range(0, nb, 4):
            nj = min(4, nb - j0)
            ps, pt = nps()
            for j in range(nj):
                P.pe(lambda e, ps=ps, j=j, j0=j0: e.transpose(ps[:, j * 128:j * 128 + n],
                                                              src[:n, (j0 + j) * 128:(j0 + j + 1) * 128],
                                                              ident[:n, :n]),
                     r=[src_tok, 'CT'], w=[pt])
            dst = dst_fn(j0, nj)
            srcv = ps[:, 0:nj * 128].rearrange("p (a b) -> p a b", a=nj)[:, :, 0:n]
            eng_alt[0] ^= 1
            if eng_alt[0]:
                P.act(lambda e, dst=dst, srcv=srcv: e.activation(out=dst, in_=srcv, func=AF.Copy),
                      r=[pt], w=[dst_tok])
            else:
                P.dve(lambda e, dst=dst, srcv=srcv: e.tensor_copy(out=dst, in_=srcv), r=[pt], w=[dst_tok])

    def bcast_rows_mm(dst, lhsT, rhs, n, ncol, r_toks, w_tok, evac):
        for c0 in range(0, ncol, 512):
            cn = min(512, ncol - c0)
            ps, pt = nps()
            P.pe(lambda e, ps=ps, c0=c0, cn=cn: e.matmul(ps[:n, :cn], lhsT, rhs[:, c0:c0 + cn],
                                                         start=True, stop=True),
                 r=list(r_toks), w=[pt])
            evac(ps[:n, :cn], dst[:n, c0:c0 + cn], pt, w_tok)

    cs = ra(0, 2048)
    scT = ra(2048, 16 * NSEQ // 2 + 8)
    scT = RA[:, 2048:2048 + 136].bitcast(BF16).rearrange("p (k s) -> p k s", k=16)
    brow = ra(4096, 12288)
    P.dma('sp', cs[:NSEQ, :], call, w=['cs'], chan='ld_a')
    P.act(lambda e: e.activation(out=cs[:NSEQ, :], in_=cs[:NSEQ, :], func=AF.Silu), r=['cs'], w=['cs'])
    transpose_to(lambda j0, nj: scT[:, j0:j0 + nj, :], cs, NSEQ, D, 'cs', 'scT')
    dump('d_cs', cs[:NSEQ, :], [NSEQ, D], F32, r=['cs'])
    dump('d_scT', scT, [128, 16, NSEQ], BF16, r=['scT'])
    ostg = [ra(16384 + i * 256, 256) for i in range(4)]
    for l in range(2):
        P.dma('sp', brow[0:1, :], ada_b[l], r=[], w=['brow'], chan='ld_a')
        for cb in range(48):
            c0 = cb * 256
            wi, wv, wt = W.get(ada_w[l][:, c0:c0 + 256].rearrange("(k p) n -> p k n", p=128), 16, 256)
            ps, pt = nps()
            for k in range(16):
                P.pe(lambda e, ps=ps, k=k, wv=wv: e.matmul(ps[:NSEQ, :256], scT[:, k, :], wv[:, k, :],
                                                            start=(k == 0), stop=False),
                     r=['scT', wt], w=[pt])
            P.pe(lambda e, ps=ps, c0=c0: e.matmul(ps[:NSEQ, :256], ones_row[:, :NSEQ], brow[0:1, c0:c0 + 256],
                                                  start=False, stop=True),
                 r=['CT', 'brow'], w=[pt])
            W.done(wi)
            sg = ostg[cb % 4]
            stok = ('ostg', cb % 4)
            P.dve(lambda e, sg=sg, ps=ps: e.tensor_copy(out=sg[:NSEQ, :], in_=ps[:NSEQ, :256]), r=[pt], w=[stok])
            P.dma('sp', mod_d[l][:, c0:c0 + 256], sg[:NSEQ, :], r=[stok], w=[('mod', l)], chan=('modst', cb % 4))


    if stop == 'ada':
        return finish()
    P.barrier()

    selP = CT[0:NSEQ, K_SELP:K_SELP + 128]
    selS = CT[0:NSEQ, K_SELS:K_SELS + 64]
    ALLT = [(i * 128, 128) for i in range(16)] + [(2048, 64)]
    MAINT = [(i * 128, 128) for i in range(8)] + [(1024, 64)]

    def make_mod(dst, l, j, n, sel, tmp, gam=None, gam_tok=None):
        tk = ('modtmp',)
        P.dma('sp', tmp[:NSEQ, :], mod_d[l][:, j * D:(j + 1) * D], r=[('mod', l)], w=[tk], chan='ld_m')
        dtok = ('modt', id(dst))
        for c0 in range(0, D, 512):
            ps, pt = nps()
            P.pe(lambda e, ps=ps, c0=c0: e.matmul(ps[:n, :512], sel, tmp[:NSEQ, c0:c0 + 512], start=True, stop=True),
                 r=[tk, 'CT'], w=[pt])
            if gam is not None:
                P.dve(lambda e, ps=ps, c0=c0: e.scalar_tensor_tensor(out=dst[:n, c0:c0 + 512], in0=ps[:n, :512], scalar=1.0,
                                                                      in1=gam[:n, c0:c0 + 512], op0=ALU.add, op1=ALU.mult),
                      r=[pt, gam_tok], w=[dtok])
            else:
                P.dve(lambda e, ps=ps, c0=c0: e.tensor_copy(out=dst[:n, c0:c0 + 512], in_=ps[:n, :512]), r=[pt], w=[dtok])
        return dtok

    def load_bc_row(dst, row, tok):
        P.dma('sp', dst, rows_d[row:row + 1, :].partition_broadcast(128), w=[tok], chan='ld_m')

    def norm_to_hT(src_rows, ttiles, hT, hT_tok, l, j_shift, j_scale, gamma_row, S, xbufs, with_mod=True, router=None, out_rows=None):
        gam = S['gam']
        gtok = ('gam',)
        load_bc_row(gam[:, :], gamma_row, gtok)
        if with_mod:
            s1p = make_mod(S['s1p'], l, j_scale, 128, selP, S['tmp'], gam, gtok)
            s2p = make_mod(S['s2p'], l, j_shift, 128, selP, S['tmp'])
            s1s = make_mod(S['s1s'], l, j_scale, 64, selS, S['tmp'], gam, gtok)
            s2s = make_mod(S['s2s'], l, j_shift, 64, selS, S['tmp'])
        def one_tile(ti, t0, n):
            xt = xbufs[ti % 2]
            xtok = ('xbuf', id(xbufs), ti % 2)
            P.dma('sp', xt[:n, :], src_rows(t0, n), r=[('xres',)], w=[xtok], chan=('ldx', ti % 2))
            stt = S['stat'][:, (ti % 2) * 4:(ti % 2) * 4 + 4]
            stok = ('stat', ti % 2)
            hb = S['h'][ti % 2]
            htok = ('hbuf', id(xbufs), ti % 2)
            P.act(lambda e, xt=xt, n=n, hb=hb: e.activation(out=hb[:n, :], in_=xt[:n, :], func=AF.Square,
                                                            accum_out=stt[:n, 0:1]),
                  r=[xtok], w=[stok, htok])
            P.act(lambda e, n=n: e.activation(out=stt[:n, 1:2], in_=stt[:n, 0:1], func=AF.Sqrt, scale=1.0 / D,
                                              bias=CT[:n, K_EPS:K_EPS + 1]),
                  r=[stok, 'CT'], w=[stok])
            P.dve(lambda e, n=n: e.reciprocal(out=stt[:n, 2:3], in_=stt[:n, 1:2]), r=[stok], w=[stok])
            if with_mod:
                is_s = (n == 64)
                s1, s1t = (S['s1s'], s1s) if is_s else (S['s1p'], s1p)
                s2, s2t = (S['s2s'], s2s) if is_s else (S['s2p'], s2p)
                P.dve(lambda e, xt=xt, n=n, hb=hb, s1=s1: e.scalar_tensor_tensor(out=hb[:n, :], in0=xt[:n, :], scalar=stt[:n, 2:3],
                                                                                 in1=s1[:n, :], op0=ALU.mult, op1=ALU.mult),
                      r=[xtok, stok, s1t], w=[htok])
                P.pool(lambda e, n=n, hb=hb, s2=s2: e.tensor_tensor(out=hb[:n, :], in0=hb[:n, :], in1=s2[:n, :], op=ALU.add),
                       r=[htok, s2t], w=[htok])
            else:
                P.dve(lambda e, xt=xt, n=n, hb=hb: e.scalar_tensor_tensor(out=hb[:n, :], in0=xt[:n, :], scalar=stt[:n, 2:3],
                                                                         in1=gam[:n, :], op0=ALU.mult, op1=ALU.mult),
                      r=[xtok, stok, gtok], w=[htok])
            if router is not None:
                router(ti, t0, n, hb, htok)
            elif hT is not None:
                transpose_to(lambda j0, nj, t0=t0, n=n: hT[:, j0:j0 + nj, t0:t0 + n], hb, n, D, htok, hT_tok)
            if out_rows is not None:
                P.dma('sp', out_rows(t0, n), hb[:n, :], r=[htok], w=[('yout',)], chan='x', is_out=True)
        for ti, (t0, n) in enumerate(ttiles):
            one_tile(ti, t0, n)

    def linear_A(hT, hT_tok, ttiles, Wap, K, col_blocks, evac):
        KT = K // 128
        for (c0, ncb) in col_blocks:
            wi, wv, wt = W.get(Wap[:, c0:c0 + ncb].rearrange("(k p) n -> p k n", p=128), KT, ncb)
            for ti, (t0, n) in enumerate(ttiles):
                ps, pt = nps()
                for k in range(KT):
                    P.pe(lambda e, ps=ps, k=k, t0=t0, n=n, wv=wv, ncb=ncb: e.matmul(
                        ps[:n, :ncb], hT[:, k, t0:t0 + n], wv[:, k, :], start=(k == 0), stop=(k == KT - 1)),
                        r=[hT_tok, wt], w=[pt])
                evac(ps, pt, ti, t0, n, c0, ncb)
            W.done(wi)

    STG = [SMALL[:, i * 256:(i + 1) * 256] for i in range(4)]
    g.stg_i = 0

    def stage_out(ps, pt, n, ncb, dst_ap, w_toks, scale_ap=None, is_out=False, accum=False, eng='dve'):
        i = g.stg_i % 4
        g.stg_i += 1
        sg = STG[i]
        stok = ('stg', i)
        if scale_ap is not None:
            P.dve(lambda e: e.tensor_scalar(out=sg[:n, :ncb], in0=ps[:n, :ncb], scalar1=scale_ap, scalar2=None, op0=ALU.mult),
                  r=[pt, 'PM'], w=[stok])
        elif eng == 'act':
            P.act(lambda e: e.activation(out=sg[:n, :ncb], in_=ps[:n, :ncb], func=AF.Copy), r=[pt], w=[stok])
        else:
            P.dve(lambda e: e.tensor_copy(out=sg[:n, :ncb], in_=ps[:n, :ncb]), r=[pt], w=[stok])
        P.dma('sp', dst_ap, sg[:n, :ncb], r=[stok], w=list(w_toks), chan=('stgc', i), is_out=is_out)

    hTall = RB[:, 0:16896].bitcast(BF16).rearrange("p (k t) -> p k t", k=16)
    S0 = dict(gam=ra(0, 2048), s1p=ra(2048, 2048), s2p=ra(4096, 2048), s1s=ra(6144, 2048), s2s=ra(8192, 2048),
              tmp=ra(10240, 2048), stat=ra(12288, 8), h=[ra(12544, 2048), ra(14592, 2048)])
    XB2 = [ra(16640, 2048), ra(18688, 2048)]
    P.dma('sp', xres, xall[TP:TA, :], w=[('xres',)], chan='ld_m')
    norm_to_hT(lambda t0, n: xall[t0:t0 + n, :], ALLT, hTall, 'hTall', 0, 0, 1, R_NM0, S0, XB2)
    dump('d_hT', hTall, [128, 16, TA], BF16, r=['hTall'])
    if stop == 'norm0':
        return finish()

    P.barrier()
    CVP = ra(0, 160).rearrange("p (c j) -> p c j", c=32)
    P.dma('sp', CVP, convp_d, w=['CVP'], chan='x')
    hst = ra(256, 4096)
    P.dma('sp', hst[:48, :], sconv.rearrange("b j c -> (b j) c"), w=['hst'], chan='x')
    histT = ra(4352, 32 * 48).rearrange("p (c r) -> p c r", c=32)
    transpose_to(lambda j0, nj: histT[:, j0:j0 + nj, :], hst, 48, 4096, 'hst', 'histT')
    PRE = [ra(6144, 2176), ra(8320, 2176)]
    PRS = [ra(10496, 112), ra(10608, 112)]
    ACC = [ra(10752, 2112), ra(12864, 2112)]
    csP = ra(14976, 96).rearrange("p (c j) -> p c j", c=32)
    csS = ra(15104, 32 * 48).rearrange("p (c r) -> p c r", c=32)
    for i in range(2):
        P.dve(lambda e, i=i: e.memset(PRE[i][:, 0:3], 0.0), w=[('pre', i)])
    TCH = [(0, 512), (512, 512), (1024, 512), (1536, 512), (2048, 64)]
    for cb in range(16):
        c0 = C_XBC + cb * 256
        wi, wv, wt = W.get(w_in0[:, c0:c0 + 256].rearrange("(k p) n -> p k n", p=128), 16, 256)
        for jj in range(2):
            ct = cb * 2 + jj
            b2 = ct % 2
            pre, prs, acc = PRE[b2], PRS[b2], ACC[b2]
            prs3 = prs.rearrange("p (b j) -> p b j", b=16)
            ptok, atok = ('pre', b2), ('acc', b2)
            for (t0, tn) in TCH:
                ps, pt = nps()
                for k in range(16):
                    P.pe(lambda e, ps=ps, k=k, t0=t0, tn=tn, wv=wv, jj=jj: e.matmul(
                        ps[:, :tn], wv[:, k, jj * 128:(jj + 1) * 128], hTall[:, k, t0:t0 + tn],
                        start=(k == 0), stop=(k == 15)), r=['hTall', wt], w=[pt])
                if t0 < TP:
                    P.dve(lambda e, ps=ps, t0=t0, tn=tn, pre=pre: e.tensor_scalar(
                        out=pre[:, 3 + t0:3 + t0 + tn], in0=ps[:, :tn], scalar1=PM[:, 0:1], scalar2=None, op0=ALU.mult),
                        r=[pt, 'PM'], w=[ptok])
                elif t0 < 2048:
                    P.act(lambda e, ps=ps, t0=t0, tn=tn, pre=pre: e.activation(out=pre[:, 3 + t0:3 + t0 + tn], in_=ps[:, :tn], func=AF.Copy),
                          r=[pt], w=[ptok])
                else:
                    P.act(lambda e, ps=ps, prs3=prs3: e.activation(out=prs3[:, :, 3:7], in_=ps[:, 0:64].rearrange("p (b j) -> p b j", b=16),
                                                                   func=AF.Copy), r=[pt], w=[ptok])
            P.dve(lambda e, prs3=prs3, ct=ct: e.tensor_copy(out=prs3[:, :, 0:3], in_=histT[:, ct, :].rearrange("p (b j) -> p b j", b=16)),
                  r=['histT'], w=[ptok])
            accs = acc[:, 2048:2112].rearrange("p (b j) -> p b j", b=16)
            P.act(lambda e, pre=pre, acc=acc, ct=ct: e.activation(out=acc[:, 0:2048], in_=pre[:, 3:2051], func=AF.Identity,
                                                                  scale=CVP[:, ct, 3:4], bias=CVP[:, ct, 4:5]),
                  r=[ptok, 'CVP'], w=[atok])
            P.act(lambda e, prs3=prs3, accs=accs, ct=ct: e.activation(out=accs, in_=prs3[:, :, 3:7], func=AF.Identity,
                                                                     scale=CVP[:, ct, 3:4], bias=CVP[:, ct, 4:5]),
                  r=[ptok, 'CVP'], w=[atok])
            for j in range(3):
                P.dve(lambda e, pre=pre, acc=acc, ct=ct, j=j: e.scalar_tensor_tensor(
                    out=acc[:, 0:2048], in0=pre[:, j:j + 2048], scalar=CVP[:, ct, j:j + 1], in1=acc[:, 0:2048],
                    op0=ALU.mult, op1=ALU.add), r=[ptok, atok, 'CVP'], w=[atok])
                P.dve(lambda e, prs3=prs3, accs=accs, ct=ct, j=j: e.scalar_tensor_tensor(
                    out=accs, in0=prs3[:, :, j:j + 4], scalar=CVP[:, ct, j:j + 1], in1=accs,
                    op0=ALU.mult, op1=ALU.add), r=[ptok, atok, 'CVP'], w=[atok])
            P.act(lambda e, acc=acc: e.activation(out=acc[:, :], in_=acc[:, :], func=AF.Silu), r=[atok], w=[atok])
            P.dma('sp', xbcT[ct * 128:(ct + 1) * 128, :], acc[:, :], r=[atok], w=[('xbcT',)], chan='x')
            P.dve(lambda e, pre=pre, ct=ct: e.tensor_copy(out=csP[:, ct, :], in_=pre[:, 2048:2051]), r=[ptok], w=['csP'])
            P.dve(lambda e, prs3=prs3, ct=ct: e.tensor_copy(out=csS[:, ct, :].rearrange("p (b j) -> p b j", b=16), in_=prs3[:, :, 4:7]),
                  r=[ptok], w=['csS'])
        W.done(wi)
    cso = ra(16640, 4096)
    for (src3, nr, dst, tok) in [(csP, 3, conv_p, 'csP'), (csS, 48, conv_s.rearrange("b j c -> (b j) c"), 'csS')]:
        for j0 in range(0, 32, 4):
            ps, pt = nps()
            for j in range(4):
                P.pe(lambda e, ps=ps, j=j, j0=j0, src3=src3, nr=nr: e.transpose(ps[:nr, j * 128:(j + 1) * 128], src3[:, j0 + j, :], ident),
                     r=[tok, 'CT'], w=[pt])
            P.dve(lambda e, ps=ps, j0=j0, nr=nr: e.tensor_copy(out=cso[:nr, j0 * 128:(j0 + 4) * 128], in_=ps[:nr, :]), r=[pt], w=['cso'])
        P.dma('sp', dst, cso[:nr, :], r=['cso'], w=[('convout', tok)], chan='x', is_out=True)
    dump('d_xbcT', None, None)
    if stop == 'conv':
        return finish()

    cols_all = [(C_DT, 32)] + [(c, 256) for c in range(C_K, C_R, 256)] + [(C_G, 16)]
    cols_main = [(c, 256) for c in range(0, 2048, 256)] + [(c, 256) for c in range(C_Q, C_K, 256)]
    cols_main += [(c, 256) for c in range(C_R, C_G, 256)]

    def evac_proj(ps, pt, ti, t0, n, c0, ncb):
        stage_out(ps, pt, n, ncb, proj[t0:t0 + n, c0:c0 + ncb], [('proj',)],
                  scale_ap=(PM[:n, 0:1] if t0 < TP else None), eng=('act' if ti % 2 else 'dve'))
    linear_A(hTall, 'hTall', ALLT, w_in0, D, cols_all, evac_proj)
    linear_A(hTall, 'hTall', ALLT[8:], w_in0, D, cols_main, evac_proj)
    if stop == 'proj':
        return finish()

    P.barrier()
    g.ps_pool = [0, 1, 2, 3]
    XB = ra(0, 4096).rearrange("p (c t) -> p c t", c=32)
    xs = ra(4096, 2048)
    bm = ra(6144, 1024)
    xdt = ra(7168, 2048)
    xdte = ra(9216, 2048)
    ya = ra(11264, 2048)
    zt = ra(13312, 2048)
    HT = ra(15360, 2048)
    sm = ra(17408, 256)
    dtp, dt_, da, acum, eA, toend, cdv, dif = [sm[:, i * 32:(i + 1) * 32] for i in range(8)]
    CBm = [ra(17664, 128), ra(17792, 128)]
    LhB = [ra(21824, 512), ra(22336, 512)]
    EbB = [ra(22848, 512), ra(23360, 512)]
    tmpy = ra(18688, 512)
    tmp2 = ra(19200, 512)
    yoacc = ra(19712, 2048)
    stat = ra(21760, 16)
    ANEG = ra(21776, 32)
    DSK = HP[:, 2, :]
    h0 = ra(21824, 2048).rearrange("p (j n) -> p j n", j=16)
    SG = rb(0, 4096).rearrange("p (a v) -> p a v", a=8)
    qk = rb(4096, 2048)
    vv = rb(6144, 2048)
    rr = rb(8192, 2048)
    glr = rb(10240, 16)
    glrT = rb(10256, 64)
    GB = [dict(la=rb(10320, 256), bcs=rb(10576, 256), q_t=rb(10832, 256), k_t=rb(11088, 256), k_e=rb(11344, 256),
               qtT=rb(11600, 128).rearrange("p (k t) -> p k t", k=2), ktT=rb(11728, 128).rearrange("p (k t) -> p k t", k=2),
               attm=rb(11856, 64), decT=rb(11920, 32)),
          dict(la=ra(17920, 256), bcs=ra(18176, 256), q_t=ra(18432, 256), k_t=ra(20736, 256), k_e=ra(20992, 256),
               qtT=ra(21248, 128).rearrange("p (k t) -> p k t", k=2), ktT=ra(21376, 128).rearrange("p (k t) -> p k t", k=2),
               attm=ra(21504, 64), decT=ra(21568, 32))]
    yb = rb(11952, 2048)
    WA2 = rb(14000, 1024)
    BAr = rb(15024, 1024)
    h0T = rb(4096, 2048)
    sstg = rb(6144, 2048)
    xdtem = rb(8192, 2048)
    cdq = rb(16048, 256).rearrange("p (j b) -> p j b", j=16)
    cdexp = rb(16304, 1024)
    S0b = rb(16304, 1024).rearrange("p (k v) -> p k v", k=2)
    P.dma('sp', WA2[:16, :], wa2_d, w=['WA2'], chan='x')
    P.dma('sp', BAr[0:1, :], rows_d[R_BA:R_BA + 1, 0:1024], w=['BAr'], chan='x')
    P.act(lambda e: e.activation(out=ANEG[:, :], in_=HP[:, 1, :], func=AF.Exp), r=['HP'], w=['ANEG'])
    P.dve(lambda e: e.tensor_scalar(out=ANEG[:, :], in0=ANEG[:, :], scalar1=-1.0, scalar2=None, op0=ALU.mult), r=['ANEG'], w=['ANEG'])
    P.dve(lambda e: e.memset(HT[:, :], 0.0), w=[('HT', q) for q in range(8)])
    P.dve(lambda e: e.memset(RB[:, 0:4096], 0.0), w=[('SG', q) for q in range(4)])
    ONEC = CT[:, K_ONE:K_ONE + 1]
    EPSC = CT[:, K_EPS:K_EPS + 1]
    SEQM = CT[:, K_SEQM:K_SEQM + 16]
    PICK = CT[:, K_PICK:K_PICK + 16]

    def rstd_from(ssq_ap, out_ap, n, width, toks):
        P.act(lambda e: e.activation(out=out_ap, in_=ssq_ap, func=AF.Sqrt, scale=1.0 / width, bias=EPSC[:n, :]), r=toks + ['CT'], w=toks)
        P.dve(lambda e: e.reciprocal(out=out_ap, in_=out_ap), r=toks, w=toks)

    def ssd_tile(t0, n, mode):
        smp = (mode == 'sample')
        g.ps_pool = [0, 1, 2, 3]
        Lm = CT[:n, (K_LS if smp else K_L):(K_LS if smp else K_L) + n]
        Rm = CT[:n, (K_RS if smp else K_R):(K_RS if smp else K_R) + n]
        CMm = CT[:n, (K_CMS if smp else K_CM):(K_CMS if smp else K_CM) + n]
        BLm = CT[:n, (K_BLKS if smp else K_BLK):(K_BLKS if smp else K_BLK) + n]
        P.dma('sp', XB[:, :, :n], xbcT[:, t0:t0 + n].rearrange("(c p) t -> p c t", p=128), r=[('xbcT',)], w=['XB'], chan='x')
        P.dma('sp', dtp[:n, :], proj[t0:t0 + n, C_DT:C_DT + 32], r=[('proj',)], w=['sm'], chan='x')
        if mode != 'prefix':
            P.dma('sp', zt[:n, :], proj[t0:t0 + n, 0:2048], r=[('proj',)], w=['zt'], chan='x')
        P.dve(lambda e: e.tensor_tensor(out=dt_[:n, :], in0=dtp[:n, :], in1=HP[:n, 0, :], op=ALU.add), r=['sm', 'HP'], w=['sm'])
        P.act(lambda e: e.activation(out=dt_[:n, :], in_=dt_[:n, :], func=AF.Exp), r=['sm'], w=['sm'])
        P.act(lambda e: e.activation(out=dt_[:n, :], in_=dt_[:n, :], func=AF.Ln, bias=ONEC[:n, :]), r=['sm', 'CT'], w=['sm'])
        P.dve(lambda e: e.tensor_tensor(out=da[:n, :], in0=dt_[:n, :], in1=ANEG[:n, :], op=ALU.mult), r=['sm', 'ANEG'], w=['sm'])
        psA, ptA = nps()
        P.pe(lambda e: e.matmul(psA[:n, 0:32], Rm, da[:n, :], start=True, stop=True), r=['sm', 'CT'], w=[ptA])
        P.pe(lambda e: e.matmul(psA[:n, 32:64], BLm, da[:n, :], start=True, stop=True), r=['sm', 'CT'], w=[ptA])
        P.dve(lambda e: e.tensor_copy(out=acum[:n, :], in_=psA[:n, 0:32]), r=[ptA], w=['sm'])
        P.act(lambda e: e.activation(out=eA[:n, :], in_=psA[:n, 0:32], func=AF.Exp), r=[ptA], w=['sm'])
        P.act(lambda e: e.activation(out=cdv[:n, :], in_=psA[:n, 32:64], func=AF.Exp), r=[ptA], w=['sm'])
        P.dve(lambda e: e.tensor_tensor(out=dif[:n, :], in0=psA[:n, 32:64], in1=acum[:n, :], op=ALU.subtract), r=[ptA, 'sm'], w=['sm'])
        P.act(lambda e: e.activation(out=toend[:n, :], in_=dif[:n, :], func=AF.Exp), r=['sm'], w=['sm'])
        for (dst, cbase, nblk, tok) in [(xs, 0, 16, 'xs'), (bm, 16, 8, 'bm')]:
            for j0 in range(0, nblk, 4):
                ps, pt = nps()
                for j in range(4):
                    P.pe(lambda e, ps=ps, j=j, j0=j0, cbase=cbase: e.transpose(ps[:n, j * 128:(j + 1) * 128], XB[:, cbase + j0 + j, :n], ident),
                         r=['XB', 'CT'], w=[pt])
                P.act(lambda e, ps=ps, j0=j0, dst=dst: e.activation(out=dst[:n, j0 * 128:(j0 + 4) * 128], in_=ps[:n, :], func=AF.Copy), r=[pt], w=[tok])
        x3 = lambda ap, c0=0, nh=32: ap[:n, c0 * 64:(c0 + nh) * 64].rearrange("p (h d) -> p h d", h=nh)
        bc3 = lambda ap, c0=0, nh=32: ap[:n, c0:c0 + nh].unsqueeze(2).to_broadcast([n, nh, 64])
        P.dve(lambda e: e.tensor_tensor(out=x3(xdt), in0=x3(xs), in1=bc3(dt_), op=ALU.mult), r=['xs', 'sm'], w=['xdt'])
        P.pool(lambda e: e.tensor_tensor(out=x3(xdte), in0=x3(xdt), in1=bc3(toend), op=ALU.mult), r=['xdt', 'sm'], w=['xdte'])
        if smp:
            ssd_sample_states(n)
        if mode != 'prefix':
            for gp in range(4):
                psy = PS[4 + 2 * (gp % 2)]
                pty = ('ps', 4 + 2 * (gp % 2))
                pso = PS[5 + 2 * (gp % 2)]
                pto = ('ps', 5 + 2 * (gp % 2))
                for gi in range(2):
                    gq = 2 * gp + gi
                    cb = CBm[gq % 2]
                    ps, pt = nps()
                    P.pe(lambda e, ps=ps, gq=gq: e.matmul(ps[:n, :n], XB[:, 16 + gq, :n], XB[:, 24 + gq, :n], start=True, stop=True), r=['XB'], w=[pt])
                    P.dve(lambda e, ps=ps, cb=cb: e.tensor_tensor(out=cb[:n, :n], in0=ps[:n, :n], in1=CMm, op=ALU.mult), r=[pt, 'CT'], w=[('CBm', gq % 2)])
                    b2 = gq % 2
                    L4 = LhB[b2][:n, 0:4 * n].rearrange("p (h t) -> p h t", h=4)
                    E4 = EbB[b2][:n, 0:4 * n].rearrange("p (h t) -> p h t", h=4)
                    h0w = [('h0', 0)] if smp else []
                    P.dve(lambda e, L4=L4, gq=gq: e.tensor_tensor(out=L4, in0=Lm.unsqueeze(1).to_broadcast([n, 4, n]),
                                                                   in1=da[:n, 4 * gq:4 * gq + 4].unsqueeze(2).to_broadcast([n, 4, n]), op=ALU.mult),
                          r=['sm', 'CT'], w=[('Lh', b2)] + h0w)
                    ps, pt = nps()
                    for hh in range(4):
                        P.pe(lambda e, ps=ps, b2=b2, hh=hh: e.matmul(ps[:n, hh * n:(hh + 1) * n], LhB[b2][:n, hh * n:(hh + 1) * n], Rm, start=True, stop=True),
                             r=[('Lh', b2), 'CT'], w=[pt])
                    P.act(lambda e, ps=ps, b2=b2: e.activation(out=EbB[b2][:n, 0:4 * n], in_=ps[:n, 0:4 * n], func=AF.Exp), r=[pt], w=[('Eb', b2)] + h0w)
                    P.pool(lambda e, E4=E4, cb=cb: e.tensor_tensor(out=E4, in0=E4, in1=cb[:n, :n].unsqueeze(1).to_broadcast([n, 4, n]), op=ALU.mult),
                           r=[('Eb', b2), ('CBm', gq % 2)], w=[('Eb', b2)])
                    for hh in range(4):
                        h = 4 * gq + hh
                        hc = (h % 8) * 64
                        P.pe(lambda e, b2=b2, h=h, hh=hh, hc=hc, psy=psy: e.matmul(psy[:n, hc:hc + 64], EbB[b2][:n, hh * n:(hh + 1) * n], xdt[:n, h * 64:(h + 1) * 64], start=True, stop=True),
                             r=[('Eb', b2), 'xdt'], w=[pty])
                    if not smp:
                        P.pe(lambda e, gq=gq, gi=gi, pso=pso: e.matmul(pso[:n, gi * 256:(gi + 1) * 256], XB[:, 24 + gq, :n], HT[:, gq * 256:(gq + 1) * 256], start=True, stop=True),
                             r=['XB', ('HT', gq)], w=[pto])
                yo_src = yoacc[:n, gp * 512:(gp + 1) * 512] if smp else pso[:n, :]
                yo_tok = 'yoacc' if smp else pto
                v8 = lambda ap: ap.rearrange("p (h d) -> p h d", h=8)
                P.dve(lambda e, yo_src=yo_src, gp=gp: e.tensor_tensor(out=v8(tmpy[:n, :]), in0=v8(yo_src), in1=bc3(eA, 8 * gp, 8), op=ALU.mult),
                      r=[yo_tok, 'sm'], w=['tmpy'])
                P.dve(lambda e, psy=psy: e.tensor_tensor(out=tmpy[:n, :], in0=tmpy[:n, :], in1=psy[:n, :], op=ALU.add), r=['tmpy', pty], w=['tmpy'])
                P.pool(lambda e, gp=gp: e.tensor_tensor(out=v8(tmp2[:n, :]), in0=x3(xs, 8 * gp, 8), in1=DSK[:n, 8 * gp:8 * gp + 8].unsqueeze(2).to_broadcast([n, 8, 64]), op=ALU.mult),
                       r=['xs', 'HP'], w=['tmp2'])
                P.pool(lambda e, gp=gp: e.tensor_tensor(out=ya[:n, gp * 512:(gp + 1) * 512], in0=tmpy[:n, :], in1=tmp2[:n, :], op=ALU.add), r=['tmpy', 'tmp2'], w=['ya'])
        if not smp:
            for gq in range(8):
                ps, pt = nps()
                P.pe(lambda e, ps=ps, gq=gq: e.matmul(ps[:, 0:256], bm[:n, gq * 128:(gq + 1) * 128], xdte[:n, gq * 256:(gq + 1) * 256], start=True, stop=True),
                     r=['bm', 'xdte'], w=[pt])
                hv = HT[:, gq * 256:(gq + 1) * 256].rearrange("p (h d) -> p h d", h=4)
                P.dve(lambda e, hv=hv, gq=gq: e.tensor_tensor(out=hv, in0=hv, in1=cdv[:, 4 * gq:4 * gq + 4].unsqueeze(2).to_broadcast([128, 4, 64]), op=ALU.mult),
                      r=[('HT', gq), 'sm'], w=[('HT', gq)])
                P.dve(lambda e, ps=ps, gq=gq: e.tensor_tensor(out=HT[:, gq * 256:(gq + 1) * 256], in0=HT[:, gq * 256:(gq + 1) * 256], in1=ps[:, 0:256], op=ALU.add),
                      r=[('HT', gq), pt], w=[('HT', gq)])
        if mode != 'prefix':
            P.act(lambda e: e.activation(out=zt[:n, :], in_=zt[:n, :], func=AF.Silu), r=['zt'], w=['zt'])
            P.dve(lambda e: e.tensor_tensor(out=ya[:n, :], in0=ya[:n, :], in1=zt[:n, :], op=ALU.mult), r=['ya', 'zt'], w=['ya'])
            P.act(lambda e: e.activation(out=zt[:n, :], in_=ya[:n, :], func=AF.Square, accum_out=stat[:n, 0:1]), r=['ya'], w=['zt', 'stat'])
            rstd_from(stat[:n, 0:1], stat[:n, 1:2], n, 2048.0, ['stat'])
            P.dve(lambda e: e.tensor_scalar(out=ya[:n, :], in0=ya[:n, :], scalar1=stat[:n, 1:2], scalar2=None, op0=ALU.mult),
                  r=['ya', 'stat'], w=['ya'])
            P.dma('sp', ymix[t0 - TP:t0 - TP + n, 0:2048], ya[:n, :], r=['ya'], w=[('ymix',)], chan='x')

    def ssd_sample_states(n):
        cdx = yoacc
        P.dve(lambda e: e.tensor_copy(out=cdx[:n, :].rearrange("p (h d) -> p h d", h=32), in_=cdv[:n, :].unsqueeze(2).to_broadcast([n, 32, 64])), r=['sm'], w=['yoacc'])
        ps, pt = nps()
        for j in range(16):
            P.pe(lambda e, ps=ps, j=j: e.matmul(ps[:, j * 16:(j + 1) * 16], cdx[:n, j * 128:(j + 1) * 128], PICK[:n, :], start=True, stop=True), r=['yoacc', 'CT'], w=[pt])
        P.dve(lambda e, ps=ps: e.tensor_copy(out=cdq, in_=ps[:, 0:256].rearrange("p (j b) -> p j b", j=16)), r=[pt], w=['cdq'])
        H0 = [h0, rb(0, 2048).rearrange("p (j n) -> p j n", j=16)]
        SSTG = [sstg, rb(2048, 2048)]
        sgall = [('SG', q) for q in range(4)]

        def ld(b):
            P.dma('sp', H0[b % 2], sssm[b].rearrange("(j q) n -> q j n", q=128), w=[('h0', b % 2)] + (sgall if b == 1 else []), chan='x')

        def one_seq(b):
            hb, hbt = H0[b % 2], ('h0', b % 2)
            sg, sgt = SSTG[b % 2], ('sstg', b % 2)
            if b + 1 < 16:
                ld(b + 1)
            for j0 in range(0, 16, 4):
                ps, pt = nps()
                for j in range(4):
                    P.pe(lambda e, ps=ps, j=j, j0=j0: e.transpose(ps[:, j * 128:(j + 1) * 128], hb[:, j0 + j, :], ident), r=[hbt, 'CT'], w=[pt])
                P.act(lambda e, ps=ps, j0=j0: e.activation(out=h0T[:, j0 * 128:(j0 + 4) * 128], in_=ps[:, :], func=AF.Copy), r=[pt], w=['h0T'])
            for gp in range(4):
                ps, pt = nps()
                for gi in range(2):
                    gq = 2 * gp + gi
                    P.pe(lambda e, ps=ps, gq=gq, gi=gi: e.matmul(ps[:n, gi * 256:(gi + 1) * 256], XB[:, 24 + gq, :n], h0T[:, gq * 256:(gq + 1) * 256], start=True, stop=True),
                         r=['XB', 'h0T'], w=[pt])
                dst = yoacc[:n, gp * 512:(gp + 1) * 512]
                if b == 0:
                    P.dve(lambda e, ps=ps, dst=dst: e.tensor_scalar(out=dst, in0=ps[:n, :], scalar1=SEQM[:n, b:b + 1], scalar2=None, op0=ALU.mult), r=[pt, 'CT', 'cdq'], w=['yoacc'])
                else:
                    P.dve(lambda e, ps=ps, dst=dst: e.scalar_tensor_tensor(out=dst, in0=ps[:n, :], scalar=SEQM[:n, b:b + 1], in1=dst, op0=ALU.mult, op1=ALU.add),
                          r=[pt, 'CT', 'yoacc'], w=['yoacc'])
            P.dve(lambda e: e.tensor_scalar(out=xdtem[:n, :], in0=xdte[:n, :], scalar1=SEQM[:n, b:b + 1], scalar2=None, op0=ALU.mult), r=['xdte', 'CT'], w=['xdtem'])
            for j0 in range(0, 16, 4):
                ps, pt = nps()
                for j in range(4):
                    jj = j0 + j
                    P.pe(lambda e, ps=ps, j=j, jj=jj: e.matmul(ps[:, j * 128:(j + 1) * 128], xdtem[:n, jj * 128:(jj + 1) * 128], bm[:n, (jj // 2) * 128:(jj // 2 + 1) * 128], start=True, stop=True),
                         r=['xdtem', 'bm'], w=[pt])
                sv = sg[:, j0 * 128:(j0 + 4) * 128].rearrange("p (j n) -> p j n", j=4)
                P.dve(lambda e, sv=sv, j0=j0: e.tensor_tensor(out=sv, in0=hb[:, j0:j0 + 4, :], in1=cdq[:, j0:j0 + 4, b:b + 1].to_broadcast([128, 4, 128]), op=ALU.mult),
                      r=[hbt, 'cdq'], w=[sgt] + (sgall if b == 1 else []))
                P.dve(lambda e, ps=ps, j0=j0: e.tensor_tensor(out=sg[:, j0 * 128:(j0 + 4) * 128], in0=sg[:, j0 * 128:(j0 + 4) * 128], in1=ps[:, :], op=ALU.add), r=[sgt, pt], w=[sgt])
            P.dma('sp', ssm_s[b].rearrange("(j q) n -> q j n", q=128), sg.rearrange("p (j n) -> p j n", j=16), r=[sgt], w=[('ssm_s',)], chan='x', is_out=True)

        ld(0)
        for b in range(16):
            one_seq(b)

    def gla_chunk(t0, n, mode):
        smp = (mode == 'sample')
        g.ps_pool = [0, 1, 2, 3, 6, 7]
        Rm = CT[:n, (K_RS if smp else K_R):(K_RS if smp else K_R) + n]
        CMm = CT[:n, (K_CMS if smp else K_CM):(K_CMS if smp else K_CM) + n]
        BLm = CT[:n, (K_BLKS if smp else K_BLK):(K_BLKS if smp else K_BLK) + n]
        if mode == 'prefix':
            P.dma('sp', qk[:n, 1024:2048], proj[t0:t0 + n, C_K:C_K + 1024], r=[('proj',)], w=['qk'], chan='x')
        else:
            P.dma('sp', qk[:n, :], proj[t0:t0 + n, C_Q:C_Q + 2048], r=[('proj',)], w=['qk'], chan='x')
        P.dma('sp', vv[:n, :], proj[t0:t0 + n, C_V:C_V + 2048], r=[('proj',)], w=['vv'], chan='x')
        P.dma('sp', glr[:n, :], proj[t0:t0 + n, C_G:C_G + 16], r=[('proj',)], w=['glr'], chan='x')
        if mode != 'prefix':
            P.dma('sp', rr[:n, :], proj[t0:t0 + n, C_R:C_R + 2048], r=[('proj',)], w=['rr'], chan='x')
            P.act(lambda e: e.activation(out=rr[:n, :], in_=rr[:n, :], func=AF.Silu), r=['rr'], w=['rr'])
        ps, pt = nps()
        P.pe(lambda e, ps=ps: e.transpose(ps[:16, 0:n], glr[:n, :], ident[:n, :n]), r=['glr', 'CT'], w=[pt])
        P.dve(lambda e, ps=ps: e.tensor_copy(out=glrT[:16, :n], in_=ps[:16, 0:n]), r=[pt], w=['glrT'])
        def head(hd):
            hb2 = hd % 2
            G_ = GB[hb2]
            la, bcs, q_t, k_t, k_e, qtT, ktT, attm, decT = (G_[k_] for k_ in 'la bcs q_t k_t k_e qtT ktT attm decT'.split())
            ps, pt = nps()
            P.pe(lambda e, ps=ps, hd=hd: e.matmul(ps[:n, 0:256], glrT[:16, :n], WA2[:16, hd * 256:(hd + 1) * 256], start=True, stop=False), r=['glrT', 'WA2'], w=[pt])
            P.pe(lambda e, ps=ps, hd=hd: e.matmul(ps[:n, 0:256], ones_row[:, :n], BAr[0:1, hd * 256:(hd + 1) * 256], start=False, stop=True), r=['CT', 'BAr'], w=[pt])
            P.act(lambda e, ps=ps: e.activation(out=la[:n, :], in_=ps[:n, 0:256], func=AF.Exp, scale=-1.0), r=[pt], w=[('la', hb2)])
            P.act(lambda e: e.activation(out=la[:n, :], in_=la[:n, :], func=AF.Ln, bias=ONEC[:n, :]), r=[('la', hb2), 'CT'], w=[('la', hb2)])
            ps, pt = nps()
            P.pe(lambda e, ps=ps: e.matmul(ps[:n, 0:256], Rm, la[:n, :], start=True, stop=True), r=[('la', hb2), 'CT'], w=[pt])
            P.pe(lambda e, ps=ps: e.matmul(ps[:n, 256:512], BLm, la[:n, :], start=True, stop=True), r=[('la', hb2), 'CT'], w=[pt])
            ps2, pt2 = nps()
            ncol = 16 if smp else 1
            rhsm = SEQM[:n, :] if smp else ONEC[:n, :]
            for kt in range(2):
                P.pe(lambda e, ps2=ps2, kt=kt: e.matmul(ps2[:, kt * 16:kt * 16 + ncol], la[:n, kt * 128:(kt + 1) * 128], rhsm, start=True, stop=True), r=[('la', hb2), 'CT'], w=[pt2])
            P.act(lambda e, ps2=ps2: e.activation(out=decT[:, :], in_=ps2[:, 0:32], func=AF.Exp, scale=-1.0 / 16.0), r=[pt2], w=[('decT', hb2)])
            P.dve(lambda e, ps=ps: e.tensor_copy(out=bcs[:n, :], in_=ps[:n, 0:256]), r=[pt], w=[('bcs', hb2)])
            qh = qk[:n, hd * 256:(hd + 1) * 256]
            kh = qk[:n, 1024 + hd * 256:1024 + (hd + 1) * 256]
            P.act(lambda e, ps=ps: e.activation(out=k_t[:n, :], in_=ps[:n, 0:256], func=AF.Exp, scale=1.0 / 16.0), r=[pt], w=[('k_t', hb2)])
            P.dve(lambda e, kh=kh: e.tensor_tensor(out=k_t[:n, :], in0=k_t[:n, :], in1=kh, op=ALU.mult), r=[('k_t', hb2), 'qk'], w=[('k_t', hb2)])
            P.dve(lambda e, ps=ps: e.tensor_tensor(out=k_e[:n, :], in0=bcs[:n, :], in1=ps[:n, 256:512], op=ALU.subtract), r=[('bcs', hb2), pt], w=[('k_e', hb2)])
            P.act(lambda e: e.activation(out=k_e[:n, :], in_=k_e[:n, :], func=AF.Exp, scale=1.0 / 16.0), r=[('k_e', hb2)], w=[('k_e', hb2)])
            P.dve(lambda e, kh=kh: e.tensor_tensor(out=k_e[:n, :], in0=k_e[:n, :], in1=kh, op=ALU.mult), r=[('k_e', hb2), 'qk'], w=[('k_e', hb2)])
            if mode != 'prefix':
                P.act(lambda e, ps=ps: e.activation(out=q_t[:n, :], in_=ps[:n, 0:256], func=AF.Exp, scale=-1.0 / 16.0), r=[pt], w=[('q_t', hb2)])
                P.dve(lambda e, qh=qh: e.scalar_tensor_tensor(out=q_t[:n, :], in0=qh, scalar=0.0625, in1=q_t[:n, :], op0=ALU.mult, op1=ALU.mult), r=[('q_t', hb2), 'qk'], w=[('q_t', hb2)])
                for (src, dstT, tk, tkT) in [(q_t, qtT, ('q_t', hb2), ('qtT', hb2)), (k_t, ktT, ('k_t', hb2), ('ktT', hb2))]:
                    ps3, pt3 = nps()
                    for kt in range(2):
                        P.pe(lambda e, ps3=ps3, kt=kt, src=src: e.transpose(ps3[:, kt * 64:kt * 64 + n], src[:n, kt * 128:(kt + 1) * 128], ident[:n, :n]), r=[tk, 'CT'], w=[pt3])
                    P.dve(lambda e, ps3=ps3, dstT=dstT: e.tensor_copy(out=dstT[:, :, :n], in_=ps3[:, 0:128].rearrange("p (k t) -> p k t", k=2)[:, :, :n]), r=[pt3], w=[tkT])
                ps4, pt4 = nps()
                for kt in range(2):
                    P.pe(lambda e, ps4=ps4, kt=kt: e.matmul(ps4[:n, :n], ktT[:, kt, :n], qtT[:, kt, :n], start=(kt == 0), stop=(kt == 1)), r=[('ktT', hb2), ('qtT', hb2)], w=[pt4])
                P.dve(lambda e, ps4=ps4: e.tensor_tensor(out=attm[:n, :n], in0=ps4[:n, :n], in1=CMm, op=ALU.mult), r=[pt4, 'CT'], w=[('attm', hb2)])
                pso = PS[4 + hd % 2]
                pto = ('ps', 4 + hd % 2)
                vh = vv[:n, hd * 512:(hd + 1) * 512]
                if not smp:
                    P.pe(lambda e, pso=pso, vh=vh: e.matmul(pso[:n, :], attm[:n, :n], vh, start=True, stop=False), r=[('attm', hb2), 'vv'], w=[pto])
                    for kt in range(2):
                        P.pe(lambda e, pso=pso, kt=kt, hd=hd: e.matmul(pso[:n, :], qtT[:, kt, :n], SG[:, hd * 2 + kt, :], start=False, stop=(kt == 1)), r=[('qtT', hb2), ('SG', hd)], w=[pto])
                    o_src, o_tok = pso[:n, :], pto
                else:
                    P.pe(lambda e, pso=pso, vh=vh: e.matmul(pso[:n, :], attm[:n, :n], vh, start=True, stop=True), r=[('attm', hb2), 'vv'], w=[pto])
                    P.dve(lambda e, pso=pso: e.tensor_copy(out=tmp2[:n, :], in_=pso[:n, :]), r=[pto], w=['tmp2'])
                    S0B = [S0b, ra(0, 1024).rearrange("p (k v) -> p k v", k=2)]
                    SOB = [yoacc[:, 0:1024], ra(1024, 1024)]
                    SOT = ['yoacc', ('so', 1)]

                    def ldg(b):
                        P.dma('sp', S0B[b % 2], sgla[b, hd * 256:(hd + 1) * 256, :].rearrange("(k p) v -> p k v", p=128), w=[('S0b', b % 2)], chan='x')

                    def one_seq(b):
                        s0, s0t = S0B[b % 2], ('S0b', b % 2)
                        sob, sot = SOB[b % 2], SOT[b % 2]
                        if b + 1 < 16:
                            ldg(b + 1)
                        ps5, pt5 = nps()
                        for kt in range(2):
                            P.pe(lambda e, ps5=ps5, kt=kt: e.matmul(ps5[:n, :], qtT[:, kt, :n], s0[:, kt, :], start=(kt == 0), stop=(kt == 1)), r=[('qtT', hb2), s0t], w=[pt5])
                        P.dve(lambda e, ps5=ps5: e.scalar_tensor_tensor(out=tmp2[:n, :], in0=ps5[:n, :], scalar=SEQM[:n, b:b + 1], in1=tmp2[:n, :], op0=ALU.mult, op1=ALU.add),
                              r=[pt5, 'tmp2', 'CT'], w=['tmp2'])
                        P.dve(lambda e: e.tensor_scalar(out=tmpy[:n, :], in0=vh, scalar1=SEQM[:n, b:b + 1], scalar2=None, op0=ALU.mult), r=['vv', 'CT'], w=['tmpy'])
                        for kt in range(2):
                            ps6, pt6 = nps()
                            P.pe(lambda e, ps6=ps6, kt=kt: e.matmul(ps6[:, :], k_e[:n, kt * 128:(kt + 1) * 128], tmpy[:n, :], start=True, stop=True), r=[('k_e', hb2), 'tmpy'], w=[pt6])
                            so = sob[:, kt * 512:(kt + 1) * 512]
                            P.dve(lambda e, ps6=ps6, kt=kt, so=so: e.scalar_tensor_tensor(out=so, in0=s0[:, kt, :], scalar=decT[:, kt * 16 + b:kt * 16 + b + 1], in1=ps6[:, :], op0=ALU.mult, op1=ALU.add),
                                  r=[s0t, ('decT', hb2), pt6], w=[sot])
                        P.dma('sp', gla_s[b, hd * 256:(hd + 1) * 256, :].rearrange("(k p) v -> p k v", p=128), sob.rearrange("p (k v) -> p k v", k=2), r=[sot], w=[('gla_s',)], chan='x', is_out=True)

                    ldg(0)
                    for b in range(16):
                        one_seq(b)
                    o_src, o_tok = tmp2[:n, :], 'tmp2'
                ybh = yb[:n, hd * 512:(hd + 1) * 512]
                P.act(lambda e, o_src=o_src, ybh=ybh, hd=hd: e.activation(out=ybh, in_=o_src, func=AF.Square, accum_out=stat[:n, 4 + hd:5 + hd]), r=[o_tok], w=['yb', 'stat'])
                rstd_from(stat[:n, 4 + hd:5 + hd], stat[:n, 8 + hd:9 + hd], n, 512.0, ['stat'])
                P.dve(lambda e, o_src=o_src, ybh=ybh, hd=hd: e.tensor_scalar(out=ybh, in0=o_src, scalar1=stat[:n, 8 + hd:9 + hd], scalar2=None, op0=ALU.mult),
                      r=[o_tok, 'stat'], w=['yb'])
                P.pool(lambda e, ybh=ybh, hd=hd: e.tensor_tensor(out=ybh, in0=ybh, in1=rr[:n, hd * 512:(hd + 1) * 512], op=ALU.mult), r=['yb', 'rr'], w=['yb'])
            if not smp:
                for kt in range(2):
                    ps7, pt7 = nps()
                    P.pe(lambda e, ps7=ps7, kt=kt, hd=hd: e.matmul(ps7[:, :], k_e[:n, kt * 128:(kt + 1) * 128], vv[:n, hd * 512:(hd + 1) * 512], start=True, stop=True), r=[('k_e', hb2), 'vv'], w=[pt7])
                    P.dve(lambda e, ps7=ps7, kt=kt, hd=hd: e.scalar_tensor_tensor(out=SG[:, hd * 2 + kt, :], in0=SG[:, hd * 2 + kt, :], scalar=decT[:, kt * 16:kt * 16 + 1], in1=ps7[:, :], op0=ALU.mult, op1=ALU.add),
                          r=[('SG', hd), ('decT', hb2), pt7], w=[('SG', hd)])
        for hd in range(4):
            head(hd)
        if mode != 'prefix':
            P.dma('sp', ymix[t0 - TP:t0 - TP + n, 2048:4096], yb[:n, :], r=['yb'], w=[('ymix',)], chan='x')

    for i in range(16):
        mode = 'prefix' if i < 8 else 'main'
        ssd_tile(i * 128, 128, mode)
        gla_chunk(i * 128, 64, mode)
        gla_chunk(i * 128 + 64, 64, mode)
        if i == 7:
            P.dve(lambda e: e.tensor_scalar(out=HT[:, :], in0=HT[:, :], scalar1=PM[:, 0:1], scalar2=None, op0=ALU.mult), r=[('HT', q) for q in range(8)] + ['PM'], w=[('HT', q) for q in range(8)])
            P.dve(lambda e: e.tensor_scalar(out=RB[:, 0:4096], in0=RB[:, 0:4096], scalar1=PM[:, 0:1], scalar2=None, op0=ALU.mult), r=[('SG', q) for q in range(4)] + ['PM'], w=[('SG', q) for q in range(4)])
    P.barrier()
    P.dma('sp', gla_p.rearrange("(a p) v -> p a v", p=128), SG, r=[('SG', q) for q in range(4)], w=[('gla_p',)], chan='x', is_out=True)
    for j0 in range(0, 16, 4):
        ps, pt = nps()
        for j in range(4):
            P.pe(lambda e, ps=ps, j=j, j0=j0: e.transpose(ps[:, j * 128:(j + 1) * 128], HT[:, (j0 + j) * 128:(j0 + j + 1) * 128], ident), r=[('HT', (j0 + j) // 2), 'CT'], w=[pt])
        P.dve(lambda e, ps=ps, j0=j0: e.tensor_copy(out=sstg[:, j0 * 128:(j0 + 4) * 128], in_=ps[:, :]), r=[pt], w=['sstg'])
    P.dma('sp', ssm_p.rearrange("(j q) n -> q j n", q=128), sstg.rearrange("p (j n) -> p j n", j=16), r=['sstg'], w=[('ssm_p',)], chan='x', is_out=True)
    P.barrier()
    ssd_tile(2048, 64, 'sample')
    P.barrier()
    gla_chunk(2048, 64, 'sample')
    g.ps_pool = list(range(8))
    if stop == 'mix':
        return finish()

    def rows_to_T(src, ncols, dstT, dst_tok, bufs, kofs=0, gains=None):
        for ti, (t0, n) in enumerate(MAINT):
            for h0c in range(0, ncols, 2048):
                b = bufs[(ti * (ncols // 2048) + h0c // 2048) % 2]
                btok = ('r2t', id(bufs), (ti * (ncols // 2048) + h0c // 2048) % 2)
                P.dma('sp', b[:n, :], src[t0:t0 + n, h0c:h0c + 2048], r=[('ymix',), ('xres',)], w=[btok], chan='x')
                if gains is not None:
                    gn, gnt = gains[h0c // 2048]
                    P.pool(lambda e, b=b, n=n, gn=gn: e.tensor_tensor(out=b[:n, :], in0=b[:n, :], in1=gn[:n, :], op=ALU.mult), r=[btok, gnt], w=[btok])
                transpose_to(lambda j0, nj, t0=t0, n=n, h0c=h0c: dstT[:, kofs + h0c // 128 + j0:kofs + h0c // 128 + j0 + nj, t0:t0 + n],
                             b, n, 2048, btok, dst_tok)

    def make_gate(l, j, Gp, Gs, tmp):
        gp_t = make_mod(Gp, l, j, 128, selP, tmp)
        gs_t = make_mod(Gs, l, j, 64, selS, tmp)
        return gp_t, gs_t

    def linear_res(hT, hT_tok, Wap, ksplit, ncols, Gp, Gs, gtoks, dg=None, NCB=256):
        if g.no_dg:
            dg = None
        for c0 in range(0, ncols, NCB):
            tl = []
            for (k0, kt) in ksplit:
                wi, wv, wt = W.get(Wap[k0 * 128:(k0 + kt) * 128, c0:c0 + NCB].rearrange("(k p) n -> p k n", p=128), kt, NCB)
                tl.append((wi, wv, wt, k0, kt))
            nk = sum(kt for (_, kt) in ksplit)
            for ti, (t0, n) in enumerate(MAINT):
                ps, pt = nps()
                kk = 0
                for (wi, wv, wt, k0, kt) in tl:
                    for k in range(kt):
                        P.pe(lambda e, ps=ps, k=k, k0=k0, t0=t0, n=n, wv=wv, kk=kk: e.matmul(
                            ps[:n, :NCB], hT[:, k0 + k, t0:t0 + n], wv[:, k, :], start=(kk == 0), stop=(kk == nk - 1)),
                            r=[hT_tok, wt], w=[pt])
                        kk += 1
                i = g.stg_i % 4
                g.stg_i += 1
                sg = STG[i]
                stok = ('stg', i)
                G = Gs if n == 64 else Gp
                gt = gtoks[1] if n == 64 else gtoks[0]
                if dg is None:
                    P.dve(lambda e, ps=ps, sg=sg, G=G, n=n, c0=c0: e.tensor_tensor(out=sg[:n, :NCB], in0=ps[:n, :NCB], in1=G[:n, c0:c0 + NCB], op=ALU.mult),
                          r=[pt, gt], w=[stok])
                else:
                    P.dve(lambda e, ps=ps, sg=sg, G=G, n=n, c0=c0, ti=ti: e.scalar_tensor_tensor(out=sg[:n, :NCB], in0=ps[:n, :NCB], scalar=dg(ti, n), in1=G[:n, c0:c0 + NCB],
                                                                                                op0=ALU.mult, op1=ALU.mult), r=[pt, gt, 'DG'], w=[stok])
                P.dma('pool', xres[t0:t0 + n, c0:c0 + NCB], sg[:n, :NCB], r=[stok, ('xres', ti, c0)], w=[('xres', ti, c0)], chan='acc', accum=True)
            for (wi, wv, wt, k0, kt) in tl:
                W.done(wi)

    def swiglu_T(hT, hT_tok, w1, w3, aT, s_tmp):
        TC = [(0, 512), (512, 512), (1024, 64)]
        for c0 in range(0, DFF, 256):
            ncb = min(256, DFF - c0)
            w1i, w1v, w1t = W.get(w1[:, c0:c0 + ncb].rearrange("(k p) n -> p k n", p=128), 16, ncb)
            w3i, w3v, w3t = W.get(w3[:, c0:c0 + ncb].rearrange("(k p) n -> p k n", p=128), 16, ncb)
            for jj in range(ncb // 128):
                f = c0 // 128 + jj
                for ci, (t0, tn) in enumerate(TC):
                    ps1, pt1 = nps()
                    for k in range(16):
                        P.pe(lambda e, ps1=ps1, k=k, t0=t0, tn=tn, jj=jj, w1v=w1v: e.matmul(ps1[:, :tn], w1v[:, k, jj * 128:(jj + 1) * 128], hT[:, k, t0:t0 + tn],
                                                                                           start=(k == 0), stop=(k == 15)), r=[hT_tok, w1t], w=[pt1])
                    ps3, pt3 = nps()
                    for k in range(16):
                        P.pe(lambda e, ps3=ps3, k=k, t0=t0, tn=tn, jj=jj, w3v=w3v: e.matmul(ps3[:, :tn], w3v[:, k, jj * 128:(jj + 1) * 128], hT[:, k, t0:t0 + tn],
                                                                                           start=(k == 0), stop=(k == 15)), r=[hT_tok, w3t], w=[pt3])
                    sb = s_tmp[(f * 3 + ci) % 2]
                    sbt = ('s_tmp', (f * 3 + ci) % 2)
                    P.act(lambda e, ps1=ps1, sb=sb, tn=tn: e.activation(out=sb[:, :tn], in_=ps1[:, :tn], func=AF.Silu), r=[pt1], w=[sbt])
                    P.dve(lambda e, ps3=ps3, sb=sb, tn=tn, f=f, t0=t0: e.tensor_tensor(out=aT[:, f, t0:t0 + tn], in0=sb[:, :tn], in1=ps3[:, :tn], op=ALU.mult),
                          r=[pt3, sbt], w=['aT'])
            W.done(w1i)
            W.done(w3i)

    P.barrier()
    yT = RB[:, 0:17408].bitcast(BF16).rearrange("p (k t) -> p k t", k=32)
    RBUF = [ra(0, 2048), ra(2048, 2048)]
    Gp, Gs, gtmp = ra(4096, 2048), ra(6144, 2048), ra(8192, 2048)
    ANORM, GNORM = ra(10240, 2048), ra(12288, 2048)
    load_bc_row(ANORM[:, :], R_AN, 'ANORM')
    load_bc_row(GNORM[:, :], R_GN, 'GNORM')
    rows_to_T(ymix, 4096, yT, 'yT', RBUF, gains=[(ANORM, 'ANORM'), (GNORM, 'GNORM')])
    gt = make_gate(0, 2, Gp, Gs, gtmp)
    linear_res(yT, 'yT', w_out0, [(0, 16), (16, 16)], D, Gp, Gs, gt)
    if stop == 'outproj':
        return finish()

    def ffn_block(l, j_shift, j_scale, j_gate, gamma_row, w1, w3, w2, router=None, dg=None, experts=None):
        P.barrier()
        hT = RB[:, 0:8704].bitcast(BF16).rearrange("p (k t) -> p k t", k=16)
        Gp2, Gs2 = rb(8704, 2048), rb(10752, 2048)
        s_tmp = [RB[:, 12800:13056].bitcast(BF16), RB[:, 13056:13312].bitcast(BF16)]
        S1 = dict(gam=ra(0, 2048), s1p=ra(2048, 2048), s2p=ra(4096, 2048), s1s=ra(6144, 2048), s2s=ra(8192, 2048),
                  tmp=ra(10240, 2048), stat=ra(12288, 8), h=[ra(12544, 2048), ra(14592, 2048)],
                  hTf=[ra(20736, 512), ra(21248, 512)])
        XB3 = [ra(16640, 2048), ra(18688, 2048)]
        norm_to_hT(lambda t0, n: xres[t0:t0 + n, :], MAINT, hT, 'hT', l, j_shift, j_scale, gamma_row, S1, XB3, router=router)
        gt2 = make_gate(l, j_gate, Gp2, Gs2, S1['tmp'])
        if router is not None:
            dump('d_DG', DG_ap[0][:, :, :], [128, 9, 8], F32, r=['DG'])
        if experts is not None and stop == 'router':
            return 'stop'
        P.barrier()
        aT = RA[:, 0:23392].bitcast(BF16).rearrange("p (f t) -> p f t", f=43)
        if experts is None:
            swiglu_T(hT, 'hT', w1, w3, aT, s_tmp)
            linear_res(aT, 'aT', w2, [(0, 15), (15, 14), (29, 14)], D, Gp2, Gs2, gt2)
        else:
            for ex in range(g.nexp_dbg):
                swiglu_T(hT, 'hT', w1[ex], w3[ex], aT, s_tmp)
                linear_res(aT, 'aT', w2[ex], [(0, 15), (15, 14), (29, 14)], D, Gp2, Gs2, gt2, dg=(lambda ti, n, ex=ex: dg(ti, n, ex)))

    ffn_block(0, 3, 4, 5, R_NF0, ffn_w1, ffn_w3, ffn_w2)
    if stop == 'ffn0':
        return finish()

    P.barrier()
    hT1 = RB[:, 0:8704].bitcast(BF16).rearrange("p (k t) -> p k t", k=16)
    S2 = dict(gam=ra(0, 2048), s1p=ra(2048, 2048), s2p=ra(4096, 2048), s1s=ra(6144, 2048), s2s=ra(8192, 2048),
              tmp=ra(10240, 2048), stat=ra(12288, 8), h=[ra(12544, 2048), ra(14592, 2048)])
    XB4 = [ra(16640, 2048), ra(18688, 2048)]
    norm_to_hT(lambda t0, n: xres[t0:t0 + n, :], MAINT, hT1, 'hT', 1, 0, 1, R_NM1, S2, XB4)
    P.barrier()

    def evac_cu(ps, pt, ti, t0, n, c0, ncb):
        i = g.stg_i % 4
        g.stg_i += 1
        sg = STG[i]
        stok = ('stg', i)
        P.act(lambda e: e.activation(out=sg[:n, :ncb], in_=ps[:n, :ncb], func=AF.Gelu), r=[pt], w=[stok])
        P.dma('sp', cu[t0:t0 + n, c0:c0 + ncb], sg[:n, :ncb], r=[stok], w=[('cu',)], chan='x')
    linear_A(hT1, 'hT', MAINT, c_w_in, D, [(c, 256) for c in range(0, 4 * D, 256)], evac_cu)
    P.barrier()
    ub, vb, vn, yc = ra(0, 4096), ra(4096, 4096), ra(8192, 4096), ra(12288, 4096)
    LG, LB = ra(16384, 4096), rb(8704, 4096)
    WST = rb(12800, 1024).rearrange("p (g t) -> p g t", g=8)
    WSTs = rb(13824, 512).rearrange("p (g t) -> p g t", g=8)
    CBS, CBSs = rb(14336, 8), rb(14344, 8)
    st1 = rb(14352, 32)
    P.dma('sp', LG[:, 0:2048], rows_d[R_LG0:R_LG0 + 1, :].partition_broadcast(128), w=['LG'], chan='x')
    P.dma('sp', LG[:, 2048:4096], rows_d[R_LG1:R_LG1 + 1, :].partition_broadcast(128), w=['LG'], chan='x')
    P.dma('sp', LB[:, 0:2048], rows_d[R_LB0:R_LB0 + 1, :].partition_broadcast(128), w=['LB'], chan='x')
    P.dma('sp', LB[:, 2048:4096], rows_d[R_LB1:R_LB1 + 1, :].partition_broadcast(128), w=['LB'], chan='x')
    P.dma('sp', WST, wsT_d, w=['WST'], chan='x')
    P.dma('sp', WSTs[:64], wsTs_d, w=['WSTs'], chan='x')
    P.dma('sp', CBS[:, :], cbs_d, w=['CBS'], chan='x')
    P.dma('sp', CBSs[:64, :], cbss_d, w=['CBS'], chan='x')
    P.dve(lambda e: e.tensor_tensor(out=WST, in0=WST, in1=CT[:, K_CM:K_CM + 128].unsqueeze(1).to_broadcast([128, 8, 128]), op=ALU.mult), r=['WST', 'CT'], w=['WST'])
    P.dve(lambda e: e.tensor_tensor(out=WSTs[:64], in0=WSTs[:64], in1=CT[:64, K_CMS:K_CMS + 64].unsqueeze(1).to_broadcast([64, 8, 64]), op=ALU.mult), r=['WSTs', 'CT'], w=['WSTs'])
    def cmix_tile(ti, t0, n):
        smp = (n == 64)
        P.dma('sp', ub[:n, :], cu[t0:t0 + n, 0:4096], r=[('cu',)], w=['ub'], chan='x')
        P.dma('sp', vb[:n, :], cu[t0:t0 + n, 4096:8192], r=[('cu',)], w=['vb'], chan='x')
        for gq in range(8):
            vg = vb[:n, gq * 512:(gq + 1) * 512]
            ng = vn[:n, gq * 512:(gq + 1) * 512]
            P.act(lambda e, vg=vg, ng=ng, gq=gq: e.activation(out=ng, in_=vg, func=AF.Copy, accum_out=st1[:n, gq:gq + 1]), r=['vb'], w=['vn', 'st1'])
            P.dve(lambda e, gq=gq: e.tensor_scalar(out=st1[:n, 8 + gq:9 + gq], in0=st1[:n, gq:gq + 1], scalar1=-1.0 / 512.0, scalar2=None, op0=ALU.mult), r=['st1'], w=['st1'])
            P.act(lambda e, vg=vg, gq=gq: e.activation(out=vg, in_=vg, func=AF.Identity, bias=st1[:n, 8 + gq:9 + gq], scale=1.0), r=['vb', 'st1'], w=['vb'])
            P.act(lambda e, vg=vg, ng=ng, gq=gq: e.activation(out=ng, in_=vg, func=AF.Square, accum_out=st1[:n, 16 + gq:17 + gq]), r=['vb'], w=['vn', 'st1'])
            P.act(lambda e, gq=gq: e.activation(out=st1[:n, 24 + gq:25 + gq], in_=st1[:n, 16 + gq:17 + gq], func=AF.Sqrt, scale=1.0 / 512.0, bias=CT[:n, K_EPS:K_EPS + 1]), r=['st1', 'CT'], w=['st1'])
            P.dve(lambda e, gq=gq: e.reciprocal(out=st1[:n, 24 + gq:25 + gq], in_=st1[:n, 24 + gq:25 + gq]), r=['st1'], w=['st1'])
            P.dve(lambda e, vg=vg, ng=ng, gq=gq: e.scalar_tensor_tensor(out=ng, in0=vg, scalar=st1[:n, 24 + gq:25 + gq], in1=LG[:n, gq * 512:(gq + 1) * 512], op0=ALU.mult, op1=ALU.mult),
                  r=['vb', 'st1', 'LG'], w=['vn'])
            P.pool(lambda e, ng=ng, gq=gq: e.tensor_tensor(out=ng, in0=ng, in1=LB[:n, gq * 512:(gq + 1) * 512], op=ALU.add), r=['vn', 'LB'], w=['vn'])
        if smp:
            P.dma('sp', cmlp_v, vn[:n, :], r=['vn'], w=[('cmlpv',)], chan='x', is_out=True)
        for gq in range(8):
            ps, pt = nps()
            lhs = WSTs[:64, gq, :] if smp else WST[:, gq, :]
            P.pe(lambda e, ps=ps, lhs=lhs, gq=gq: e.matmul(ps[:n, :], lhs, vn[:n, gq * 512:(gq + 1) * 512], start=True, stop=True), r=['WST', 'WSTs', 'vn'], w=[pt])
            bsc = (CBSs if smp else CBS)[:n, gq:gq + 1]
            P.dve(lambda e, ps=ps, gq=gq, bsc=bsc: e.scalar_tensor_tensor(out=yc[:n, gq * 512:(gq + 1) * 512], in0=ps[:n, :], scalar=bsc, in1=ub[:n, gq * 512:(gq + 1) * 512], op0=ALU.add, op1=ALU.mult),
                  r=[pt, 'CBS', 'ub'], w=['yc'])
        P.dma('sp', ymix[t0:t0 + n, :], yc[:n, :], r=['yc'], w=[('ymix',)], chan='x')
    for ti, (t0, n) in enumerate(MAINT):
        cmix_tile(ti, t0, n)
    P.barrier()
    yT1 = RB[:, 0:17408].bitcast(BF16).rearrange("p (k t) -> p k t", k=32)
    rows_to_T(ymix, 4096, yT1, 'yT', RBUF)
    gt1 = make_gate(1, 2, Gp, Gs, gtmp)
    linear_res(yT1, 'yT', c_w_out, [(0, 16), (16, 16)], D, Gp, Gs, gt1)
    if stop == 'mixc':
        return finish()

    if 'noalloc' in g.rflags:
        DG = rb(13312, 72).rearrange("p (a b) -> p a b", a=9)
        WR = rb(13440, 128).rearrange("p (a b) -> p a b", a=16)
        RS = rb(13568, 64)
    else:
        DG = st.enter_context(nc.sbuf_tensor("DG", [128, 9, 8], F32))
        WR = st.enter_context(nc.sbuf_tensor("WR", [128, 16, 8], F32))
        RS = st.enter_context(nc.sbuf_tensor("RS", [128, 64], F32))
    if 'nowr' not in g.rflags:
        P.dma('sp', WR[:, :, :], router_d.rearrange("(k p) e -> p k e", p=128), w=['WR'], chan='x')
    hTm = RB[:, 0:8704].bitcast(BF16).rearrange("p (k t) -> p k t", k=16)
    HTF = [ra(20736, 512), ra(21248, 512)]

    def router(ti, t0, n, hb, htok):
        psr, ptr = PS[7], ('ps', 7)
        for j0 in range(0, 16, 4):
            ps, pt = nps()
            for j in range(4):
                P.pe(lambda e, ps=ps, j=j, j0=j0: e.transpose(ps[:, j * 128:j * 128 + n], hb[:n, (j0 + j) * 128:(j0 + j + 1) * 128], ident[:n, :n]), r=[htok, 'CT'], w=[pt])
            hf = HTF[(j0 // 4) % 2]
            hft = ('HTF', (j0 // 4) % 2)
            src = ps[:, 0:512].rearrange("p (a b) -> p a b", a=4)[:, :, 0:n]
            hf3 = hf.rearrange("p (a b) -> p a b", a=4)[:, :, 0:n]
            P.act(lambda e, src=src, hf3=hf3: e.activation(out=hf3, in_=src, func=AF.Copy), r=[pt], w=[hft])
            P.dve(lambda e, hf3=hf3, j0=j0: e.tensor_copy(out=hTm[:, j0:j0 + 4, t0:t0 + n], in_=hf3), r=[hft], w=['hT'])
            for j in range(4 if g.rmode < 2 else 0):
                P.pe(lambda e, j=j, j0=j0, hf=hf: e.matmul(psr[:n, 0:8], hf[:, j * 128:j * 128 + n], WR[:, j0 + j, :], start=(j0 + j == 0), stop=(j0 + j == 15)), r=[hft, 'WR'], w=[ptr])
        if g.rmode >= 1:
            if 'nodg' not in g.rflags:
                P.dve(lambda e, ti=ti: e.memset(DG[:n, ti, :], 0.125), w=['DG'])
            return
        lg, mx, ngv, ex, msk, den = RS[:n, 0:8], RS[:n, 8:16], RS[:n, 16:17], RS[:n, 24:32], RS[:n, 32:40], RS[:n, 40:41]
        P.dve(lambda e: e.tensor_copy(out=lg, in_=psr[:n, 0:8]), r=[ptr], w=['RS'])
        P.dve(lambda e: e.tensor_reduce(out=mx[:, 0:1], in_=lg, axis=mybir.AxisListType.X, op=ALU.max), r=['RS'], w=['RS'])
        P.dve(lambda e: e.tensor_scalar(out=msk, in0=lg, scalar1=mx[:, 0:1], scalar2=-1e30, op0=ALU.is_equal, op1=ALU.mult), r=['RS'], w=['RS'])
        P.dve(lambda e: e.tensor_tensor(out=msk, in0=msk, in1=lg, op=ALU.add), r=['RS'], w=['RS'])
        P.dve(lambda e: e.tensor_reduce(out=mx[:, 1:2], in_=msk, axis=mybir.AxisListType.X, op=ALU.max), r=['RS'], w=['RS'])
        P.dve(lambda e: e.tensor_scalar(out=ngv, in0=mx[:, 0:1], scalar1=-1.0, scalar2=None, op0=ALU.mult), r=['RS'], w=['RS'])
        P.act(lambda e: e.activation(out=ex, in_=lg, func=AF.Exp, bias=ngv, scale=1.0), r=['RS'], w=['RS'])
        P.dve(lambda e: e.tensor_scalar(out=msk, in0=lg, scalar1=mx[:, 1:2], scalar2=None, op0=ALU.is_ge), r=['RS'], w=['RS'])
        P.dve(lambda e: e.tensor_tensor(out=ex, in0=ex, in1=msk, op=ALU.mult), r=['RS'], w=['RS'])
        P.dve(lambda e: e.tensor_reduce(out=den, in_=ex, axis=mybir.AxisListType.X, op=ALU.add), r=['RS'], w=['RS'])
        P.dve(lambda e: e.reciprocal(out=den, in_=den), r=['RS'], w=['RS'])
        P.dve(lambda e, ti=ti: e.tensor_scalar(out=DG[:n, ti, :], in0=ex, scalar1=den, scalar2=None, op0=ALU.mult), r=['RS'], w=['DG'])

    g.ps_pool = list(range(7))
    DG_ap.append(DG)
    rr_ = ffn_block(1, 3, 4, 5, R_NF1, moe_w1, moe_w3, moe_w2, router=(None if ('nocb' in g.rflags and stop == 'router') else router), dg=(lambda ti, n, ex: DG[:n, ti, ex:ex + 1]), experts=True)
    g.ps_pool = list(range(8))
    if stop in ('moe', 'router'):
        return finish()
    P.barrier()
    norm_to_hT(lambda t0, n: xres[t0:t0 + n, :], MAINT, None, None, 1, 0, 0, R_NFIN, S2, XB4, with_mod=False,
               out_rows=lambda t0, n: y_out[t0:t0 + n, :])
    return finish()

def make_consts():
    c = np.zeros((128, NCONST), np.float32)
    i = np.arange(128)
    c[:, K_ID:K_ID + 128] = np.eye(128)
    u = i[:, None]; t = i[None, :]
    c[:, K_L:K_L + 128] = (u > t)
    c[:, K_R:K_R + 128] = (u <= t)
    c[:, K_CM:K_CM + 128] = (u <= t)
    c[:, K_BLK:K_BLK + 128] = 1.0
    same = (u // 4 == t // 4)
    c[:, K_LS:K_LS + 128] = (u > t) & same
    c[:, K_RS:K_RS + 128] = (u <= t) & same
    c[:, K_CMS:K_CMS + 128] = (u <= t) & same
    c[:, K_BLKS:K_BLKS + 128] = same
    c[:, K_SEQM:K_SEQM + 16] = (i[:, None] // 4 == np.arange(16)[None, :])
    c[:, K_PICK:K_PICK + 16] = (i[:, None] == 4 * np.arange(16)[None, :])
    c[:, K_ONE:K_ONE + 128] = 1.0
    c[0, K_SELP:K_SELP + 128] = 1.0
    c[1:, K_SELP:K_SELP + 128] = 0.0
    for b in range(16):
        c[1 + b, K_SELS + 4 * b:K_SELS + 4 * b + 4] = 1.0
    c[:, K_EPS] = EPS
    return c


def prep_core(inp, c):
    f = np.float32
    seq, half = c // 2, c % 2
    xp = inp['x_prompt'][seq]
    main = xp[half * TP:(half + 1) * TP]
    prefix = xp[0:TP]
    xs = inp['x_sample'][16 * c:16 * c + 16].reshape(TS, D)
    xall = np.concatenate([prefix, main, xs], 0)
    call = np.concatenate([inp['c_prompt'][seq:seq + 1], inp['c_sample'][16 * c:16 * c + 16]], 0)
    rows = np.zeros((NROW, D), f)
    rows[R_NM0] = inp['norm_mix0'][0]; rows[R_NF0] = inp['norm_ffn0'][0]
    rows[R_AN] = inp['a_norm'][0]; rows[R_GN] = inp['gla_norm'][0]
    rows[R_NM1] = inp['norm_mix1'][0]; rows[R_NF1] = inp['norm_ffn1'][0]; rows[R_NFIN] = inp['norm_f']
    rows[R_LG0] = inp['c_ln_g'][0][:D]; rows[R_LG1] = inp['c_ln_g'][0][D:]
    rows[R_LB0] = inp['c_ln_b'][0][:D]; rows[R_LB1] = inp['c_ln_b'][0][D:]
    rows[R_BA, :1024] = inp['gla_ba'][0]
    cw = inp['conv_w'][0]; cb = inp['conv_b'][0]
    convp = np.zeros((128, 32, 5), f)
    convp[:, :, :4] = cw.reshape(4, 32, 128).transpose(2, 1, 0)
    convp[:, :, 4] = cb.reshape(32, 128).T
    hp = np.zeros((128, 3, 32), f)
    hp[:, 0] = inp['dt_bias'][0]; hp[:, 1] = inp['a_log'][0]; hp[:, 2] = inp['d_skip'][0]
    ws = inp['c_ws'][0]; bs = inp['c_bs'][0]
    wsT = np.ascontiguousarray(ws.transpose(2, 0, 1))
    wsTs = np.zeros((64, 8, 64), f)
    cbss = np.zeros((64, 8), f)
    for b in range(16):
        wsTs[4 * b:4 * b + 4, :, 4 * b:4 * b + 4] = wsT[:4, :, :4]
        cbss[4 * b:4 * b + 4] = bs[:, :4].T
    m = dict(
        xall=xall, call=call, pmask=np.full((128, 1), float(half), f), consts=make_consts(), rows=rows,
        convp=convp, hp=hp, gla_wa2=inp['gla_wa2'][0],
        sssm=inp['state_ssm'][0, 16 * c:16 * c + 16].reshape(16, 2048, 128),
        sconv=inp['state_conv'][0, 16 * c:16 * c + 16],
        sgla=inp['state_gla'][0, 16 * c:16 * c + 16].reshape(16, 1024, 512),
        ada_w0=inp['ada_w0'][0], ada_w1=inp['ada_w1'][0], ada_b0=inp['ada_b0'], ada_b1=inp['ada_b1'],
        w_in0=inp['w_in0'][0], w_out0=inp['w_out0'][0], ffn_w1=inp['ffn_w1'][0], ffn_w3=inp['ffn_w3'][0],
        ffn_w2=inp['ffn_w2'][0], c_w_in=inp['c_w_in'][0], c_w_out=inp['c_w_out'][0],
        wsT=wsT, wsTs=wsTs, cbs=np.ascontiguousarray(bs.T), cbss=cbss, router_w=inp['router_w'][0],
    )
    for i in range(NEXP):
        m['moe_w1_%d' % i] = inp['moe_w1'][0][i]
        m['moe_w3_%d' % i] = inp['moe_w3'][0][i]
        m['moe_w2_%d' % i] = inp['moe_w2'][0][i]
    return {k: np.ascontiguousarray(np.asarray(v, dtype=np.float32)) for k, v in m.items()}


def kernel(**inputs):
    inp = {k: np.asarray(v) for k, v in inputs.items()}
    nc = build()
    in_maps = [prep_core(inp, c) for c in range(8)]
    res = run_bass_kernel_spmd(nc, in_maps, core_ids=list(range(8)))
    outs = res.results
    f = np.float32
    y_prompt = np.zeros((4, 2048, D), f)
    y_sample = np.zeros((128, 4, D), f)
    ssm_prompt = np.zeros((1, 4, 32, 64, 128), f)
    conv_prompt = np.zeros((1, 4, 3, 4096), f)
    gla_prompt = np.zeros((1, 4, 4, 256, 512), f)
    ssm_sample = np.zeros((1, 128, 32, 64, 128), f)
    conv_sample = np.zeros((1, 128, 3, 4096), f)
    gla_sample = np.zeros((1, 128, 4, 256, 512), f)
    cmlp = np.zeros((1, 128, 4, 8, 512), f)
    for c in range(8):
        o = outs[c]
        seq, half = c // 2, c % 2
        y_prompt[seq, half * TP:(half + 1) * TP] = o['y_out'][:TP]
        y_sample[16 * c:16 * c + 16] = o['y_out'][TP:].reshape(16, 4, D)
        if half == 1:
            ssm_prompt[0, seq] = o['ssm_p'].reshape(32, 64, 128)
            conv_prompt[0, seq] = o['conv_p']
            gla_prompt[0, seq] = o['gla_p'].reshape(4, 256, 512)
        ssm_sample[0, 16 * c:16 * c + 16] = o['ssm_s'].reshape(16, 32, 64, 128)
        conv_sample[0, 16 * c:16 * c + 16] = o['conv_s']
        gla_sample[0, 16 * c:16 * c + 16] = o['gla_s'].reshape(16, 4, 256, 512)
        cmlp[0, 16 * c:16 * c + 16] = o['cmlp_v'].reshape(16, 4, 8, 512)
    return (y_prompt, y_sample, ssm_prompt, conv_prompt, gla_prompt, ssm_sample, conv_sample, gla_sample, cmlp)
```
